# Optimizing a Trainium2 kernel written in Bass

```python
import math
import jax, jax.numpy as jnp
from jax import lax
import numpy as np

D_MODEL = 1024
BATCH = 8
SEQ = 4096
DEPTH = 4

P_DIM = 256
D_FF = 2816
ML_HEADS = 4
ML_QK_DIM = 64
ML_V_DIM = 128
ML_CHUNK = 64
SW_Q_HEADS = 8
SW_KV_HEADS = 2
SW_HEAD_DIM = 64
WINDOW = 128
REL_BUCKETS = 32
REL_MAX_DIST = 128
EPS = 1e-6

ML_QK_W = ML_HEADS * ML_QK_DIM
ML_V_W = ML_HEADS * ML_V_DIM
SW_Q_W = SW_Q_HEADS * SW_HEAD_DIM
SW_KV_W = SW_KV_HEADS * SW_HEAD_DIM
SPLITS = (ML_QK_W, ML_QK_W, ML_V_W, ML_V_W, ML_HEADS, ML_HEADS, SW_Q_W, SW_KV_W, SW_KV_W, D_MODEL, D_MODEL)
D_IN = 2 * ML_QK_W + 2 * ML_V_W + 2 * ML_HEADS + SW_Q_W + 2 * SW_KV_W + 2 * D_MODEL

kernel_name = "hybrid_mlstm_swa_sink_macaron"


def rmsnorm(x, g):
    xf = x.astype(jnp.float32)
    y = xf * lax.rsqrt(jnp.mean(xf * xf, axis=-1, keepdims=True) + EPS)
    return (y * g.astype(jnp.float32)).astype(x.dtype)


def swiglu_ffn(x, wi, wo):
    g, u = jnp.split(x @ wi, 2, axis=-1)
    return (jax.nn.silu(g) * u) @ wo


def t5_bucket(dist):
    max_exact = REL_BUCKETS // 2
    d = np.maximum(dist, 0)
    large = max_exact + (np.log(np.maximum(d, 1) / max_exact) / np.log(REL_MAX_DIST / max_exact)
                         * (REL_BUCKETS - max_exact)).astype(np.int32)
    large = np.minimum(large, REL_BUCKETS - 1)
    return np.where(d < max_exact, d, large).astype(np.int32)


def mlstm_chunkwise(q, k, v, i_pre, f_pre):
    B, S, H, dk = q.shape
    dv = v.shape[-1]
    L = ML_CHUNK
    NC = S // L
    f32 = jnp.float32
    qc = q.astype(f32).reshape(B, NC, L, H, dk)
    kc = (k.astype(f32) / math.sqrt(dk)).reshape(B, NC, L, H, dk)
    vc = v.astype(f32).reshape(B, NC, L, H, dv)
    log_i = jnp.swapaxes(i_pre.astype(f32).reshape(B, NC, L, H), 2, 3)
    log_f = jnp.swapaxes(jax.nn.log_sigmoid(f_pre.astype(f32)).reshape(B, NC, L, H), 2, 3)
    b = jnp.cumsum(log_f, axis=-1)
    g = b[..., -1]
    a = g[..., None] - b + log_i
    m_loc = jnp.max(a, axis=-1)
    w = jnp.exp(a - m_loc[..., None])
    C_loc = jnp.einsum('bnhl,bnlhk,bnlhv->bnhkv', w, kc, vc)
    n_loc = jnp.einsum('bnhl,bnlhk->bnhk', w, kc)

    def step(carry, xs):
        C, n, m = carry
        g_c, m_c, C_c, n_c = xs
        m_new = jnp.maximum(g_c + m, m_c)
        s_old = jnp.exp(g_c + m - m_new)
        s_new = jnp.exp(m_c - m_new)
        C_new = s_old[..., None, None] * C + s_new[..., None, None] * C_c
        n_new = s_old[..., None] * n + s_new[..., None] * n_c
        return (C_new, n_new, m_new), (C, n, m)

    init = (jnp.zeros((B, H, dk, dv), f32), jnp.zeros((B, H, dk), f32), jnp.zeros((B, H), f32))
    xs = (jnp.moveaxis(g, 1, 0), jnp.moveaxis(m_loc, 1, 0), jnp.moveaxis(C_loc, 1, 0), jnp.moveaxis(n_loc, 1, 0))
    _, (C_prev, n_prev, m_prev) = lax.scan(step, init, xs)
    C_prev = jnp.moveaxis(C_prev, 0, 1)
    n_prev = jnp.moveaxis(n_prev, 0, 1)
    m_prev = jnp.moveaxis(m_prev, 0, 1)

    causal = jnp.tril(jnp.ones((L, L), dtype=bool))
    D = b[..., :, None] - b[..., None, :] + log_i[..., None, :]
    D = jnp.where(causal, D, -jnp.inf)
    e = b + m_prev[..., None]
    m_t = jnp.maximum(jnp.max(D, axis=-1), e)
    W = jnp.exp(D - m_t[..., None]) * jnp.einsum('bnthd,bnshd->bnhts', qc, kc)
    s_inter = jnp.exp(e - m_t)
    num = (jnp.einsum('bnhts,bnshv->bnthv', W, vc)
           + jnp.swapaxes(s_inter, 2, 3)[..., None] * jnp.einsum('bnthd,bnhdv->bnthv', qc, C_prev))
    den = jnp.sum(W, axis=-1) + s_inter * jnp.einsum('bnthd,bnhd->bnht', qc, n_prev)
    denom = jnp.maximum(jnp.abs(den), jnp.exp(-m_t))
    h = num / jnp.swapaxes(denom, 2, 3)[..., None]
    return h.reshape(B, S, H, dv).astype(q.dtype)


def swa_sink_attention(q, k, v, q_gain, k_gain, sinks, rel_bias):
    B, S, Hq, d = q.shape
    Hkv = k.shape[2]
    G = Hq // Hkv
    Wn = WINDOW
    NB = S // Wn
    f32 = jnp.float32
    q = rmsnorm(q, q_gain)
    k = rmsnorm(k, k_gain)
    qb = q.astype(f32).reshape(B, NB, Wn, Hkv, G, d) * (d ** -0.5)
    kb = k.astype(f32).reshape(B, NB, Wn, Hkv, d)
    vb = v.astype(f32).reshape(B, NB, Wn, Hkv, d)
    shift = lambda t: jnp.concatenate([jnp.zeros_like(t[:, :1]), t[:, :-1]], axis=1)
    kk = jnp.concatenate([shift(kb), kb], axis=2)
    vv = jnp.concatenate([shift(vb), vb], axis=2)
    logits = jnp.einsum('bnqkgd,bnskd->bnkgqs', qb, kk)
    dist = np.arange(Wn)[:, None] + Wn - np.arange(2 * Wn)[None, :]
    in_window = (dist >= 0) & (dist < Wn)
    bias = rel_bias.astype(f32)[t5_bucket(dist)]
    bias = jnp.transpose(bias, (2, 0, 1)).reshape(Hkv, G, Wn, 2 * Wn)
    key_pos = np.arange(NB)[:, None] * Wn - Wn + np.arange(2 * Wn)[None, :]
    valid = in_window[None] & (key_pos >= 0)[:, None, :]
    logits = jnp.where(valid[None, :, None, None], logits + bias, -jnp.inf)
    sink = sinks.astype(f32).reshape(Hkv, G)[None, None, :, :, None, None]
    m = jnp.maximum(jnp.max(logits, axis=-1, keepdims=True), sink)
    pr = jnp.exp(logits - m)
    denom = jnp.sum(pr, axis=-1, keepdims=True) + jnp.exp(sink - m)
    out = jnp.einsum('bnkgqs,bnskd->bnqkgd', pr / denom, vv)
    return out.reshape(B, S, Hq * d).astype(q.dtype)


def setup_inputs(seed: int = 0) -> dict:
    key = jax.random.key(seed)
    ks = jax.random.split(key, 24)
    f32 = jnp.float32
    nrm = lambda k, shape, fan_in: jax.random.normal(k, shape, f32) * (fan_in ** -0.5)
    gain = lambda k, shape: 1.0 + 0.05 * jax.random.normal(k, shape, f32)
    return {
        "x": jax.random.normal(ks[0], (BATCH, SEQ, D_MODEL), f32),
        "p": jax.random.normal(ks[1], (DEPTH, BATCH, SEQ, P_DIM), f32),
        "ffn1_norm": gain(ks[2], (DEPTH, D_MODEL)),
        "ffn1_wi": nrm(ks[3], (DEPTH, D_MODEL, 2 * D_FF), D_MODEL),
        "ffn1_wo": nrm(ks[4], (DEPTH, D_FF, D_MODEL), D_FF),
        "mix_norm": gain(ks[5], (DEPTH, D_MODEL)),
        "w_in": nrm(ks[6], (DEPTH, D_MODEL, D_IN), D_MODEL),
        "b_igate": 0.1 * jax.random.normal(ks[7], (DEPTH, ML_HEADS), f32),
        "b_fgate": jnp.linspace(3.0, 6.0, ML_HEADS, dtype=f32)[None, :] + 0.1 * jax.random.normal(ks[8], (DEPTH, ML_HEADS), f32),
        "ml_out_norm": gain(ks[9], (DEPTH, ML_HEADS, ML_V_DIM)),
        "q_norm": gain(ks[10], (DEPTH, SW_HEAD_DIM)),
        "k_norm": gain(ks[11], (DEPTH, SW_HEAD_DIM)),
        "sinks": 0.5 * jax.random.normal(ks[12], (DEPTH, SW_Q_HEADS), f32),
        "rel_bias": 0.5 * jax.random.normal(ks[13], (REL_BUCKETS, SW_Q_HEADS), f32),
        "w_a": nrm(ks[14], (DEPTH, ML_V_W, D_MODEL), ML_V_W),
        "w_b": nrm(ks[15], (DEPTH, SW_Q_W, D_MODEL), SW_Q_W),
        "w_out": nrm(ks[16], (DEPTH, D_MODEL, D_MODEL), D_MODEL),
        "ffn2_norm": gain(ks[17], (DEPTH, D_MODEL)),
        "ffn2_wi": nrm(ks[18], (DEPTH, D_MODEL, 2 * D_FF), D_MODEL),
        "ffn2_wo": nrm(ks[19], (DEPTH, D_FF, D_MODEL), D_FF),
        "ple_norm": gain(ks[20], (DEPTH, D_MODEL)),
        "w_ple_gate": nrm(ks[21], (DEPTH, D_MODEL, D_MODEL), D_MODEL),
        "w_ple": nrm(ks[22], (DEPTH, P_DIM, D_MODEL), P_DIM),
    }


def reference(x, p, ffn1_norm, ffn1_wi, ffn1_wo, mix_norm, w_in, b_igate, b_fgate, ml_out_norm,
              q_norm, k_norm, sinks, rel_bias, w_a, w_b, w_out, ffn2_norm, ffn2_wi, ffn2_wo,
              ple_norm, w_ple_gate, w_ple):
    B, S, _ = x.shape
    offsets = [int(o) for o in np.cumsum(SPLITS)[:-1]]
    for i in range(DEPTH):
        x = x + 0.5 * swiglu_ffn(rmsnorm(x, ffn1_norm[i]), ffn1_wi[i], ffn1_wo[i])
        u = rmsnorm(x, mix_norm[i])
        z = u @ w_in[i]
        mq, mk, mv, mo, mi, mf, sq, sk, sv, ga, gb = jnp.split(z, offsets, axis=-1)
        hA = mlstm_chunkwise(mq.reshape(B, S, ML_HEADS, ML_QK_DIM),
                             mk.reshape(B, S, ML_HEADS, ML_QK_DIM),
                             mv.reshape(B, S, ML_HEADS, ML_V_DIM),
                             mi + b_igate[i], mf + b_fgate[i])
        hA = rmsnorm(hA, ml_out_norm[i]).reshape(B, S, ML_V_W)
        yA = (jax.nn.sigmoid(mo) * hA) @ w_a[i]
        hB = swa_sink_attention(sq.reshape(B, S, SW_Q_HEADS, SW_HEAD_DIM),
                                sk.reshape(B, S, SW_KV_HEADS, SW_HEAD_DIM),
                                sv.reshape(B, S, SW_KV_HEADS, SW_HEAD_DIM),
                                q_norm[i], k_norm[i], sinks[i], rel_bias)
        yB = hB @ w_b[i]
        mixed = jax.nn.sigmoid(ga) * yA + jax.nn.sigmoid(gb) * yB
        x = x + mixed @ w_out[i]
        x = x + 0.5 * swiglu_ffn(rmsnorm(x, ffn2_norm[i]), ffn2_wi[i], ffn2_wo[i])
        gate = jax.nn.sigmoid(rmsnorm(x, ple_norm[i]) @ w_ple_gate[i])
        x = x + gate * (p[i] @ w_ple[i])
    return x
```

```python
import numpy as np
import concourse.bass as bass
import concourse.mybir as mybir
from concourse.bass_utils import run_bass_kernel_spmd

F32 = mybir.dt.float32
BF16 = mybir.dt.bfloat16
AF = mybir.ActivationFunctionType
ALU = mybir.AluOpType

D = 1024
KC = 8
FF = 2816
NJ = 22
PD = 256
EPS = 1e-6
OFF_MQ, OFF_MK, OFF_MV, OFF_MO, OFF_MI, OFF_MF = 0, 256, 512, 1024, 1536, 1540
OFF_SQ, OFF_SK, OFF_SV, OFF_GA, OFF_GB = 1544, 2056, 2184, 2312, 3336
D_IN = 4360
NB_COLS = 1416
RING = 5
SLOT = 3072
T = 512
NBLK = T // 128


class Buf:
    __slots__ = ("name", "w", "r", "dsem", "dcount", "live")

    def __init__(self, name):
        self.name = name
        self.w = None
        self.r = {}
        self.dsem = None
        self.dcount = 0
        self.live = False

    def add_read(self, ev):
        s, v = ev
        if self.r.get(s, 0) < v:
            self.r[s] = v


class Sched:
    def __init__(self, nc):
        self.nc = nc
        self.sems = []
        self.engs = {}
        for name in ("pe", "act", "dve", "pool", "sp"):
            sid = self.new_sem("s_" + name)
            self.engs[name] = dict(sem=sid, count=0, waited={}, prog=[])

    def new_sem(self, name):
        self.sems.append(self.nc.alloc_semaphore(name))
        return len(self.sems) - 1

    def _deps(self, E, reads, writes):
        need = {}
        for b in reads:
            if b.w is not None and need.get(b.w[0], 0) < b.w[1]:
                need[b.w[0]] = b.w[1]
        for b in writes:
            if b.w is not None and need.get(b.w[0], 0) < b.w[1]:
                need[b.w[0]] = b.w[1]
            for s, v in b.r.items():
                if need.get(s, 0) < v:
                    need[s] = v
        waits = []
        for s, v in need.items():
            if E["waited"].get(s, 0) >= v:
                continue
            E["waited"][s] = v
            waits.append((s, v))
        return waits

    def op(self, eng, fn, reads=(), writes=()):
        E = self.engs[eng]
        waits = self._deps(E, reads, writes)
        E["count"] += 1
        ev = (E["sem"], E["count"])
        E["prog"].append((waits, fn, (E["sem"], 1)))
        for b in reads:
            b.add_read(ev)
        for b in writes:
            b.w = ev
            b.r = {}

    def pe(self, fn, reads=(), writes=()):
        self.op("pe", fn, reads, writes)

    def act(self, fn, reads=(), writes=()):
        self.op("act", fn, reads, writes)

    def dve(self, fn, reads=(), writes=()):
        self.op("dve", fn, reads, writes)

    def dma(self, queue, fn, owner, reads=(), writes=(), track=()):
        E = self.engs[queue]
        waits = self._deps(E, reads, writes)
        if owner.dsem is None:
            owner.dsem = self.new_sem("d_" + owner.name)
        owner.dcount += 16
        ev = (owner.dsem, owner.dcount)
        E["prog"].append((waits, fn, (owner.dsem, 16)))
        for b in reads:
            b.add_read(ev)
        for b in writes:
            b.w = ev
            b.r = {}
        for b in track:
            b.w = ev

    def final_wait(self, eng, bufs):
        E = self.engs[eng]
        waits = self._deps(E, bufs, bufs)
        E["prog"].append((waits, None, None))

    def emit(self):
        nc = self.nc
        sems = self.sems
        with nc.Block() as block:
            def run(name):
                def body(e):
                    for waits, fn, inc in self.engs[name]["prog"]:
                        for s, v in waits:
                            e.wait_ge(sems[s], v)
                        if fn is not None:
                            ins = fn()
                            ins.then_inc(sems[inc[0]], inc[1])
                return body
            block.tensor(run("pe"))
            block.scalar(run("act"))
            block.vector(run("dve"))
            block.gpsimd(run("pool"))
            block.sync(run("sp"))


def param_layout(L):
    off = {}
    o = 0
    off["ng"] = o; o += L * 4 * 8
    off["bg"] = o; o += L * 8
    off["mlg"] = o; o += L * 512
    off["gqk"] = o; o += L * 2
    off["snk"] = o; o += L * 8
    off["n"] = o
    return off


def pack_params(L, ffn1_norm, mix_norm, ffn2_norm, ple_norm, b_igate, b_fgate, ml_out_norm,
                q_norm, k_norm, sinks):
    off = param_layout(L)
    P = np.zeros((128, off["n"]), np.float32)
    ng = np.stack([ffn1_norm, mix_norm, ffn2_norm, ple_norm], axis=1)
    ng = ng.reshape(L, 4, 8, 128).transpose(3, 0, 1, 2).reshape(128, -1)
    P[:, off["ng"]:off["ng"] + L * 32] = ng
    bg = np.concatenate([b_igate, b_fgate], axis=1).reshape(1, -1)
    P[:, off["bg"]:off["bg"] + L * 8] = np.broadcast_to(bg, (128, L * 8))
    P[:, off["mlg"]:off["mlg"] + L * 512] = np.broadcast_to(ml_out_norm.reshape(1, -1), (128, L * 512))
    gqk = np.stack([q_norm, k_norm], axis=1)
    gqk = np.concatenate([gqk, gqk], axis=2).transpose(2, 0, 1).reshape(128, -1)
    P[:, off["gqk"]:off["gqk"] + L * 2] = gqk
    P[:, off["snk"]:off["snk"] + L * 8] = np.broadcast_to(sinks.reshape(1, -1), (128, L * 8))
    return P


def t5_bucket(dist):
    max_exact = 16
    d = np.maximum(dist, 0)
    large = max_exact + (np.log(np.maximum(d, 1) / max_exact) / np.log(128 / max_exact)
                         * (32 - max_exact)).astype(np.int32)
    large = np.minimum(large, 31)
    return np.where(d < max_exact, d, large).astype(np.int32)


def make_consts(rel_bias):
    s = np.arange(128)[:, None]
    t = np.arange(128)[None, :]
    tri = (s <= t).astype(np.float32)
    c = {}
    c["ident"] = np.eye(128, dtype=np.float32)
    c["tri4"] = np.tile(tri, (1, 4))
    bo = np.zeros((128, 128), np.float32)
    bo[:64, :64] = 1.0
    bo[64:, 64:] = 1.0
    c["blockones"] = bo
    dist_prev = t + 128 - s
    dist_cur = t - s
    valid = np.stack([(s > t), (s <= t)]).astype(np.float32)
    idx = np.stack([t5_bucket(np.clip(dist_prev, 0, 127)), t5_bucket(np.clip(dist_cur, 0, 127))])
    g = np.asarray(rel_bias, np.float32)[idx]
    c["biasg"] = np.ascontiguousarray(g.transpose(1, 0, 3, 2)).reshape(128, 2 * 8 * 128)
    c["maskc"] = np.ascontiguousarray(np.broadcast_to(valid.transpose(1, 0, 2)[:, :, None, :],
                                                      (128, 2, 8, 128))).reshape(128, 2 * 8 * 128)
    return c


def build_program(S_LEN, L):
    NT = S_LEN // T
    nc = bass.Bass("TRN2", target_bir_lowering=False)
    S = Sched(nc)
    off = param_layout(L)

    def dram_in(name, shape, dt=F32):
        return nc.dram_tensor(name, list(shape), dt, kind="ExternalInput").ap()

    x_d = dram_in("x", [S_LEN, D])
    p_d = dram_in("p", [L, S_LEN, PD])
    out_d = nc.dram_tensor("out", [S_LEN, D], F32, kind="ExternalOutput").ap()
    wi_d = [dram_in("ffn1_wi", [L, D, 2 * FF]), dram_in("ffn2_wi", [L, D, 2 * FF])]
    wo_d = [dram_in("ffn1_wo", [L, FF, D]), dram_in("ffn2_wo", [L, FF, D])]
    win_d = dram_in("w_in", [L, D, D_IN])
    wa_d = dram_in("w_a", [L, 512, D])
    wb_d = dram_in("w_b", [L, 512, D])
    wout_d = dram_in("w_out", [L, D, D])
    wpg_d = dram_in("w_ple_gate", [L, D, D])
    wple_d = dram_in("w_ple", [L, PD, D])
    par_d = dram_in("params", [128, off["n"]])
    ident_d = dram_in("ident", [128, 128])
    tri4_d = dram_in("tri4", [128, 512])
    bo_d = dram_in("blockones", [128, 128])
    biasg_d = dram_in("biasg", [128, 2048])
    maskc_d = dram_in("maskc", [128, 2048])

    def scr(name, shape):
        return nc.dram_tensor(name, list(shape), BF16).ap()

    sc = []
    for l in range(L):
        e = {}
        e["wi"] = [scr(f"sc_wi{f}_{l}", [NJ, 128, 2048]) for f in range(2)]
        e["wo"] = [scr(f"sc_wo{f}_{l}", [8, 128, NJ * 128]) for f in range(2)]
        e["winA"] = scr(f"sc_winA_{l}", [5, 128, 2048])
        e["mix"] = scr(f"sc_mix_{l}", [8, 128, 3072])
        e["wout"] = scr(f"sc_wout_{l}", [8, 128, 1024])
        e["ple"] = scr(f"sc_ple_{l}", [8, 128, 1280])
        e["winB"] = scr(f"sc_winB_{l}", [128, 8 * NB_COLS])
        e["b"] = {k: Buf(f"b_{k}_{l}") for k in ("wi0", "wi1", "wo0", "wo1", "winA", "mix", "wout", "ple", "winB")}
        sc.append(e)

    def cast(dst, src, owner):
        S.dma("pool", lambda: nc.gpsimd.dma_start(out=dst, in_=src), owner, track=[owner])

    def kview(w2d, c0, n):
        return w2d.rearrange("(k p) c -> p k c", p=128)[:, :, c0:c0 + n]

    def emit_casts(l):
        e = sc[l]
        for f in range(2):
            for j in range(NJ):
                rec = e["wi"][f][j].rearrange("p (k h n) -> p k h n", k=8, h=2)
                for h in range(2):
                    cast(rec[:, :, h, :], kview(wi_d[f][l], h * FF + j * 128, 128), e["b"][f"wi{f}"])
            for c in range(8):
                rec = e["wo"][f][c].rearrange("p (j n) -> p j n", n=128)
                cast(rec, kview(wo_d[f][l], c * 128, 128), e["b"][f"wo{f}"])
            if f == 0:
                emit_casts_mixer(l)
        for c in range(8):
            rec = e["ple"][c]
            cast(rec[:, 0:1024].rearrange("p (k n) -> p k n", n=128), kview(wpg_d[l], c * 128, 128), e["b"]["ple"])
            cast(rec[:, 1024:1280].rearrange("p (k n) -> p k n", n=128), kview(wple_d[l], c * 128, 128), e["b"]["ple"])

    def emit_casts_mixer(l):
        e = sc[l]
        w = win_d[l]
        rb = e["winB"].rearrange("p (k c) -> p k c", c=NB_COLS)
        for (d0, s0, n) in ((0, OFF_MV, 512), (512, OFF_MO, 512), (1024, OFF_MK, 256), (1280, OFF_MI, 8), (1288, OFF_SV, 128)):
            cast(rb[:, :, d0:d0 + n], kview(w, s0, n), e["b"]["winB"])
        recs = e["winA"]
        def ra(r, i):
            return recs[r].rearrange("p (i k n) -> p i k n", i=2, k=8)[:, i]
        cast(ra(0, 0), kview(w, OFF_SQ, 128), e["b"]["winA"])
        cast(ra(0, 1), kview(w, OFF_SQ + 128, 128), e["b"]["winA"])
        cast(ra(1, 0), kview(w, OFF_MQ, 128), e["b"]["winA"])
        cast(ra(1, 1), kview(w, OFF_MQ + 128, 128), e["b"]["winA"])
        cast(ra(2, 0), kview(w, OFF_SQ + 256, 128), e["b"]["winA"])
        cast(ra(2, 1), kview(w, OFF_SQ + 384, 128), e["b"]["winA"])
        cast(ra(3, 0), kview(w, OFF_MK, 128), e["b"]["winA"])
        cast(ra(3, 1), kview(w, OFF_MK + 128, 128), e["b"]["winA"])
        for g in range(2):
            for half in range(2):
                cast(ra(4, g)[:, :, half * 64:(half + 1) * 64], kview(w, OFF_SK + g * 64, 64), e["b"]["winA"])
        for c in range(8):
            rec = e["mix"][c]
            cast(rec[:, 0:1024].rearrange("p (k n) -> p k n", n=128), kview(w, OFF_GA + c * 128, 128), e["b"]["mix"])
            cast(rec[:, 1024:2048].rearrange("p (k n) -> p k n", n=128), kview(w, OFF_GB + c * 128, 128), e["b"]["mix"])
            cast(rec[:, 2048:2560].rearrange("p (k n) -> p k n", n=128), kview(wa_d[l], c * 128, 128), e["b"]["mix"])
            cast(rec[:, 2560:3072].rearrange("p (k n) -> p k n", n=128), kview(wb_d[l], c * 128, 128), e["b"]["mix"])
        for c in range(8):
            cast(e["wout"][c].rearrange("p (k n) -> p k n", n=128), kview(wout_d[l], c * 128, 128), e["b"]["wout"])

    def sb(name, shape, dt=F32):
        return nc.alloc_sbuf_tensor("sb_" + name, list(shape), dt)

    XT = sb("XT", [128, KC, T]); XTb = [Buf(f"XT{k}") for k in range(KC)]
    XN = sb("XN", [128, KC, T], BF16); XNb = Buf("XN")
    H = sb("H", [128, NJ, T], BF16); Hb = [Buf(f"H{j}") for j in range(NJ)]
    XSown = Buf("XS")
    ring = [sb(f"ring{i}", [128, SLOT], BF16) for i in range(RING)]
    ringb = [Buf(f"ring{i}") for i in range(RING)]
    WB = sb("WB", [128, KC, NB_COLS], BF16); WBb = Buf("WB")
    PAR = sb("PAR", [128, off["n"]]); PARb = Buf("PAR")
    identf = sb("identf", [128, 128]); identb = sb("identb", [128, 128], BF16)
    tri4 = sb("tri4", [128, 512])
    onesb = sb("onesb", [128, 128], BF16); onesf = sb("onesf", [128, 128])
    blockones = sb("blockones", [128, 128], BF16)
    EB = sb("EB", [128, 2, 8, 128], BF16)
    CONb = Buf("consts")
    GQ8 = sb("GQ8", [128, L]); ES = sb("ES", [128, L * 8])
    MQT = sb("MQTz", [128, 4, T], BF16); MQTb = Buf("MQT")
    MKT = sb("MKT", [128, 2, T], BF16); MKTb = Buf("MKT")
    SQT = sb("SQT", [128, 4, T], BF16); SQTb = Buf("SQT")
    SKT = sb("SKTz", [128, 2, 2, T], BF16); SKTb = Buf("SKT")
    SKP = sb("SKP", [128, L, 2, 2, 128], BF16); SKPb = [Buf(f"SKP{l}") for l in range(L)]
    SVA = sb("SVA", [128, NBLK, 2, 65], BF16); SVAb = [Buf(f"SVA{b}") for b in range(NBLK)]
    SVP = sb("SVP", [128, L, 2, 65], BF16); SVPb = [Buf(f"SVP{l}") for l in range(L)]
    HBT = sb("HBT", [128, 4, T], BF16); HBTb = Buf("HBT")
    CS = sb("CS", [128, L, 2, 129]); CSb = [Buf(f"CS{l}") for l in range(L)]
    CSB = sb("CSB", [128, L, 2, 129], BF16); CSBb = [Buf(f"CSB{l}") for l in range(L)]
    PS_ = sb("PS_", [128, NBLK, PD]); PSb_ = Buf("PS_")
    PT = sb("PT", [128, 2, T], BF16); PTb = Buf("PT")
    XS = H[:].rearrange("p j t -> p (j t)").bitcast(F32)[:, 0:NBLK * D].rearrange("p (b d) -> p b d", b=NBLK)
    XSl = Hb[0:16]
    MIXT = lambda c: H[:, 8 + c, :]
    HAGT = lambda k: H[:, 16 + k, :]

    class Pool2:
        def __init__(self, name, n, shape, dt=F32):
            self.t = [sb(f"{name}{i}", shape, dt) for i in range(n)]
            self.b = [Buf(f"{name}{i}") for i in range(n)]
            self.i = 0
        def get(self):
            i = self.i
            self.i = (i + 1) % len(self.t)
            return self.t[i], self.b[i]

    tmpA = Pool2("tmpA", 3, [128, T])
    tmpB = Pool2("tmpB", 3, [128, T])
    tmpH = Pool2("tmpH", 3, [128, T], BF16)
    MVs = Pool2("MVs", 2, [128, 512])
    SMO = Pool2("SMO", 2, [128, 512])
    MKK = Pool2("MKK", 4, [128, 256], BF16)
    VPP = Pool2("VPP", 4, [128, 4, 129], BF16)
    GSP = Pool2("GSP", 4, [128, 512])
    PTM = Pool2("PTM", 4, [128, 512], BF16)
    PTS = Pool2("PTS", 4, [128, 512], BF16)
    HTK = Pool2("HTK", 4, [128, 512], BF16)
    SM = Pool2("SM", 48, [128, 16])

    banks = [nc.alloc_psum_tensor(f"bank{i}", [128, 512], F32) for i in range(7)]
    bankT = nc.alloc_psum_tensor("bankT", [128, 1024], BF16)
    bankb = [Buf(f"bank{i}") for i in range(7)]
    _bt = Buf("bankT"); bankTb = [_bt, _bt]
    bstate = {"i": 0}

    def nb():
        for _ in range(7):
            i = bstate["i"]
            bstate["i"] = (i + 1) % 7
            if not bankb[i].live:
                bankb[i].live = True
                return banks[i], bankb[i]
        raise RuntimeError("no free PSUM bank")

    def fb(*bs):
        for b in bs:
            b.live = False

    plan = []
    for ti in range(NT):
        for l in range(L):
            for f in range(2):
                if f == 1:
                    for r in range(5):
                        plan.append((f"winA{r}", sc[l]["winA"][r], 2048, sc[l]["b"]["winA"]))
                    for c in range(8):
                        plan.append((f"mix{c}", sc[l]["mix"][c], 3072, sc[l]["b"]["mix"]))
                    for c in range(8):
                        plan.append((f"wout{c}", sc[l]["wout"][c], 1024, sc[l]["b"]["wout"]))
                for j in range(NJ):
                    plan.append((f"wi{f}_{j}", sc[l]["wi"][f][j], 2048, sc[l]["b"][f"wi{f}"]))
                for c in range(8):
                    plan.append((f"wo{f}_{c}", sc[l]["wo"][f][c], NJ * 128, sc[l]["b"][f"wo{f}"]))
            for c in range(8):
                plan.append((f"ple{c}", sc[l]["ple"][c], 1280, sc[l]["b"]["ple"]))
    import os as _os2
    _kph = _os2.environ.get("KPH", "cfmp")
    def _keep(k):
        if k[:2] in ("wi", "wo") and k[2] in "01" and k[3] == "_":
            return "f" in _kph
        if k.startswith("ple"):
            return "p" in _kph
        return "m" in _kph
    plan = [e_ for e_ in plan if _keep(e_[0])]
    wst = {"pos": 0, "loaded": 0}

    def w_load_upto(n):
        while wst["loaded"] <= n and wst["loaded"] < len(plan):
            m = wst["loaded"]
            key, src, ne, sbuf_ = plan[m]
            slot = m % RING
            S.dma("sp", lambda slot=slot, src=src, ne=ne: nc.sync.dma_start(out=ring[slot][:, 0:ne], in_=src),
                  ringb[slot], reads=[sbuf_], writes=[ringb[slot]])
            wst["loaded"] += 1

    def w_next(key):
        n = wst["pos"]
        assert plan[n][0] == key, (plan[n][0], key)
        w_load_upto(n)
        wst["pos"] += 1
        return ring[n % RING], ringb[n % RING], n

    def w_done(n):
        w_load_upto(n + RING)

    def setup():
        stg = XS[:, 0:2, :].rearrange("p a d -> p (a d)")
        stg2 = XS[:, 2:4, :].rearrange("p a d -> p (a d)")
        def ld(dst, src, owner):
            ws = XSl if owner is XSown else [owner]
            S.dma("sp", lambda: nc.sync.dma_start(out=dst, in_=src), owner, writes=ws)
        stb = stb2 = XSown
        ld(PAR[:], par_d, PARb)
        ld(identf[:], ident_d, CONb)
        ld(tri4[:], tri4_d, CONb)
        ld(stg[:, 0:128], bo_d, stb)
        ld(stg2, maskc_d, stb2)
        S.dve(lambda: nc.vector.tensor_copy(out=identb[:], in_=identf[:]), reads=[CONb], writes=[CONb])
        S.dve(lambda: nc.vector.tensor_copy(out=blockones[:], in_=stg[:, 0:128]), reads=XSl, writes=[CONb])
        S.dve(lambda: nc.vector.memset(onesb[:], 1.0), writes=[CONb])
        S.dve(lambda: nc.vector.memset(onesf[:], 1.0), writes=[CONb])
        ld(stg, biasg_d, stb)
        S.act(lambda: nc.scalar.activation(out=stg, in_=stg, func=AF.Exp), reads=XSl, writes=XSl)
        S.dve(lambda: nc.vector.tensor_tensor(out=EB[:].rearrange("p a h t -> p (a h t)"), in0=stg, in1=stg2, op=ALU.mult),
              reads=XSl, writes=[CONb])
        S.dve(lambda: nc.vector.tensor_scalar(out=GQ8[:], in0=PAR[:, off["gqk"]:off["gqk"] + 2 * L:2], scalar1=0.125, scalar2=None, op0=ALU.mult),
              reads=[PARb], writes=[CONb])
        S.act(lambda: nc.scalar.activation(out=ES[:], in_=PAR[:, off["snk"]:off["snk"] + 8 * L], func=AF.Exp), reads=[PARb], writes=[CONb])
        S.dve(lambda: nc.vector.memset(CS[:], 0.0), writes=CSb)
        S.dve(lambda: nc.vector.memset(CSB[:], 0.0), writes=CSBb)
        S.dve(lambda: nc.vector.memset(SVA[:], 1.0), writes=SVAb)
        S.dve(lambda: nc.vector.memset(SVP[:], 1.0), writes=SVPb)
        S.dve(lambda: nc.vector.memset(SKP[:], 0.0), writes=SKPb)
        S.dve(lambda: nc.vector.memset(SKT[:], 0.0), writes=[SKTb])
        S.dve(lambda: nc.vector.memset(MQT[:], 0.0), writes=[MQTb])

    def prologue(ti):
        ns = None
        S.dma("sp", lambda: nc.sync.dma_start(out=XS, in_=x_d[ti * T:(ti + 1) * T, :].rearrange("(b p) d -> p b d", p=128)),
              XSown, writes=XSl)
        for kc in range(KC):
            if kc == 0:
                ns = norm_begin((0, 0))
            bk, bb = nb()
            def tr(kc=kc, bk=bk):
                for b in range(NBLK):
                    ins = nc.tensor.transpose(bk[:, b * 128:(b + 1) * 128], XS[:, b, kc * 128:(kc + 1) * 128], identf[:])
                return ins
            S.pe(tr, reads=XSl + [CONb], writes=[bb])
            if kc % 2 == 0:
                S.act(lambda kc=kc, bk=bk: nc.scalar.copy(out=XT[:, kc, :], in_=bk[:]), reads=[bb], writes=[XTb[kc]])
            else:
                S.dve(lambda kc=kc, bk=bk: nc.vector.tensor_copy(out=XT[:, kc, :], in_=bk[:]), reads=[bb], writes=[XTb[kc]])
            fb(bb)
            norm_feed(ns, kc)
        return ns

    def epilogue(ti):
        for b in range(NBLK):
            for half in range(2):
                bk, bb = nb()
                def tr(b=b, half=half, bk=bk):
                    for q in range(4):
                        ins = nc.tensor.transpose(bk[:, q * 128:(q + 1) * 128], XT[:, half * 4 + q, b * 128:(b + 1) * 128], identf[:])
                    return ins
                S.pe(tr, reads=XTb[half * 4:half * 4 + 4] + [CONb], writes=[bb])
                if half == 0:
                    S.act(lambda b=b, half=half, bk=bk: nc.scalar.copy(out=XS[:, b, half * 512:(half + 1) * 512], in_=bk[:]), reads=[bb], writes=XSl)
                else:
                    S.dve(lambda b=b, half=half, bk=bk: nc.vector.tensor_copy(out=XS[:, b, half * 512:(half + 1) * 512], in_=bk[:]), reads=[bb], writes=XSl)
                fb(bb)
        S.dma("sp", lambda: nc.sync.dma_start(out=out_d[ti * T:(ti + 1) * T, :].rearrange("(b p) d -> p b d", p=128), in_=XS),
              XSown, reads=XSl)

    def rstd_from(bk, bb, scale):
        t1, t1b = tmpA.get()
        S.act(lambda: nc.scalar.activation(out=t1[:], in_=bk[:], func=AF.Ln, scale=scale, bias=EPS_AP[:]), reads=[bb, CONb], writes=[t1b])
        t2, t2b = tmpA.get()
        S.act(lambda: nc.scalar.activation(out=t2[:], in_=t1[:], func=AF.Exp, scale=-0.5), reads=[t1b], writes=[t2b])
        return t2, t2b

    RS = sb("RS", [128, T]); RSb = Buf("RS")

    def norm_begin(defer=None):
        bk, bb = nb()
        return dict(bk=bk, bb=bb, pend=None, n=0, defer=defer)

    def norm_flush(ns):
        if ns["pend"] is None:
            return
        sq, sqb = ns["pend"]
        k = ns["n"]
        bk, bb = ns["bk"], ns["bb"]
        S.pe(lambda: nc.tensor.matmul(bk[:], lhsT=onesb[:], rhs=sq[:], start=(k == 0), stop=(k == KC - 1)), reads=[sqb, CONb], writes=[bb])
        ns["n"] += 1
        ns["pend"] = None

    def norm_feed(ns, c):
        if ns is None:
            return
        sq, sqb = tmpH.get()
        S.act(lambda: nc.scalar.activation(out=sq[:], in_=XT[:, c, :], func=AF.Square), reads=[XTb[c]], writes=[sqb])
        if ns["defer"] is not None:
            l_, w_ = ns["defer"]
            g_ = off["ng"] + (l_ * 4 + w_) * 8 + c
            S.dve(lambda: nc.vector.tensor_scalar(out=XN[:, c, :], in0=XT[:, c, :], scalar1=PAR[:, g_:g_ + 1], scalar2=None, op0=ALU.mult),
                  reads=[XTb[c], PARb], writes=[XNb])
        norm_flush(ns)
        ns["pend"] = (sq, sqb)

    def norm_end(ns, l, which):
        norm_flush(ns)
        assert ns["n"] == KC
        bk, bb = ns["bk"], ns["bb"]
        if ns["defer"] is not None:
            assert ns["defer"] == (l, which)
            t1, t1b = tmpA.get()
            S.act(lambda: nc.scalar.activation(out=t1[:], in_=bk[:], func=AF.Ln, scale=1.0 / D, bias=EPS_AP[:]), reads=[bb, CONb], writes=[t1b])
            S.act(lambda: nc.scalar.activation(out=RS[:], in_=t1[:], func=AF.Exp, scale=-0.5), reads=[t1b], writes=[RSb])
            fb(bb)
            return True
        rs, rsb = rstd_from(bk, bb, 1.0 / D)
        fb(bb)
        g0 = off["ng"] + (l * 4 + which) * 8
        def sc_():
            for kc in range(KC):
                ins = nc.vector.scalar_tensor_tensor(out=XN[:, kc, :], in0=XT[:, kc, :], scalar=PAR[:, g0 + kc:g0 + kc + 1], in1=rs[:],
                                                     op0=ALU.mult, op1=ALU.mult)
            return ins
        S.dve(sc_, reads=XTb + [rsb, PARb], writes=[XNb])
        return False

    def ffn(l, f, ns_in, ti=None):
        if f == 1:
            S.dma("sp", lambda: nc.sync.dma_start(out=PS_[:], in_=p_d[l, ti * T:(ti + 1) * T, :].rearrange("(b p) d -> p b d", p=128)), PSb_, writes=[PSb_])
        dfr = norm_end(ns_in, l, 0 if f == 0 else 2)
        ns = None
        for j in range(NJ):
            rt, rb, n = w_next(f"wi{f}_{j}")
            bg, bgb = nb()
            bu, bub = nb()
            def mmg(rt=rt, bg=bg):
                for kc in range(KC):
                    ins = nc.tensor.matmul(bg[:], lhsT=rt[:, kc * 256:kc * 256 + 128], rhs=XN[:, kc, :], start=(kc == 0), stop=(kc == KC - 1))
                return ins
            def mmu(rt=rt, bu=bu):
                for kc in range(KC):
                    ins = nc.tensor.matmul(bu[:], lhsT=rt[:, kc * 256 + 128:kc * 256 + 256], rhs=XN[:, kc, :], start=(kc == 0), stop=(kc == KC - 1))
                return ins
            S.pe(mmg, reads=[rb, XNb], writes=[bgb])
            S.pe(mmu, reads=[rb, XNb], writes=[bub])
            w_done(n)
            sg, sgb = tmpA.get()
            if dfr:
                gs_, gsb_ = tmpB.get()
                S.dve(lambda gs_=gs_, bg=bg: nc.vector.tensor_tensor(out=gs_[:], in0=bg[:], in1=RS[:], op=ALU.mult), reads=[bgb, RSb], writes=[gsb_])
                S.act(lambda sg=sg, gs_=gs_: nc.scalar.activation(out=sg[:], in_=gs_[:], func=AF.Silu), reads=[gsb_], writes=[sgb])
                us_, usb_ = tmpB.get()
                S.dve(lambda us_=us_, bu=bu: nc.vector.tensor_tensor(out=us_[:], in0=bu[:], in1=RS[:], op=ALU.mult), reads=[bub, RSb], writes=[usb_])
                S.dve(lambda j=j, us_=us_, sg=sg: nc.vector.tensor_tensor(out=H[:, j, :], in0=us_[:], in1=sg[:], op=ALU.mult),
                      reads=[usb_, sgb], writes=[Hb[j]])
            else:
                S.act(lambda sg=sg, bg=bg: nc.scalar.activation(out=sg[:], in_=bg[:], func=AF.Silu), reads=[bgb], writes=[sgb])
                S.dve(lambda j=j, bu=bu, sg=sg: nc.vector.tensor_tensor(out=H[:, j, :], in0=bu[:], in1=sg[:], op=ALU.mult),
                      reads=[bub, sgb], writes=[Hb[j]])
            fb(bgb, bub)
        for c in range(8):
            rt, rb, n = w_next(f"wo{f}_{c}")
            if c == 0:
                ns = norm_begin((l, 3) if f == 1 else None)
            bk, bb = nb()
            def mmo(rt=rt, bk=bk):
                for j in range(NJ):
                    ins = nc.tensor.matmul(bk[:], lhsT=rt[:, j * 128:(j + 1) * 128], rhs=H[:, j, :], start=(j == 0), stop=(j == NJ - 1))
                return ins
            S.pe(mmo, reads=[rb] + Hb, writes=[bb])
            w_done(n)
            S.dve(lambda c=c, bk=bk: nc.vector.scalar_tensor_tensor(out=XT[:, c, :], in0=bk[:], scalar=0.5, in1=XT[:, c, :], op0=ALU.mult, op1=ALU.add),
                  reads=[bb, XTb[c]], writes=[XTb[c]])
            fb(bb)
            norm_feed(ns, c)
        return ns

    def projA(rt, rb, i, bk, bb):
        def mm():
            for kc in range(KC):
                ins = nc.tensor.matmul(bk[:], lhsT=rt[:, (i * 8 + kc) * 128:(i * 8 + kc + 1) * 128], rhs=XN[:, kc, :], start=(kc == 0), stop=(kc == KC - 1))
            return ins
        S.pe(mm, reads=[rb, XNb], writes=[bb])

    def qk_wave_start(rt, rb):
        raws = []
        for i in range(2):
            bk, bb = nb()
            projA(rt, rb, i, bk, bb)
            sq, sqb = tmpH.get()
            S.act(lambda sq=sq, bk=bk: nc.scalar.activation(out=sq[:], in_=bk[:], func=AF.Square), reads=[bb], writes=[sqb])
            raws.append((bk, bb, sq, sqb))
        return raws

    def qk_wave_finish(raws, fin, dstb):
        for i in range(2):
            bk, bb, sq, sqb = raws[i]
            b2, b2b = nb()
            S.pe(lambda b2=b2, sq=sq: nc.tensor.matmul(b2[:], lhsT=blockones[:], rhs=sq[:], start=True, stop=True), reads=[sqb, CONb], writes=[b2b])
            rs, rsb = rstd_from(b2, b2b, 1.0 / 64)
            fb(b2b)
            S.dve(lambda i=i, bk=bk, rs=rs: fin(i, bk, rs), reads=[bb, rsb, CONb, PARb], writes=[dstb])
            fb(bb)

    def mixer(ti, l, ns_in):
        norm_end(ns_in, l, 1)
        ns = None
        S.dma("sp", lambda: nc.sync.dma_start(out=WB[:].rearrange("p k c -> p (k c)"), in_=sc[l]["winB"]), WBb,
              reads=[sc[l]["b"]["winB"]], writes=[WBb])
        rt, rb, n = w_next("winA0")
        raws = qk_wave_start(rt, rb)
        w_done(n)
        rt, rb, n = w_next("winA1")
        for i in range(2):
            bk, bb = nb()
            projA(rt, rb, i, bk, bb)
            def fmq(i=i, bk=bk):
                nc.scalar.mul(out=MQT[0:64, 2 * i, :], in_=bk[0:64, :], mul=0.125)
                return nc.scalar.mul(out=MQT[64:128, 2 * i + 1, :], in_=bk[64:128, :], mul=0.125)
            S.act(fmq, reads=[bb], writes=[MQTb])
            fb(bb)
        w_done(n)
        def fin_q(c0):
            return lambda i, bk, rs: nc.vector.scalar_tensor_tensor(out=SQT[:, c0 + i, :], in0=bk[:], scalar=GQ8[:, l:l + 1], in1=rs[:], op0=ALU.mult, op1=ALU.mult)
        qk_wave_finish(raws, fin_q(0), SQTb)
        rt, rb, n = w_next("winA2")
        raws = qk_wave_start(rt, rb)
        w_done(n)
        rt, rb, n = w_next("winA3")
        for i in range(2):
            bk, bb = nb()
            projA(rt, rb, i, bk, bb)
            S.dve(lambda i=i, bk=bk: nc.vector.tensor_copy(out=MKT[:, i, :], in_=bk[:]), reads=[bb], writes=[MKTb])
            fb(bb)
        w_done(n)
        qk_wave_finish(raws, fin_q(2), SQTb)
        rt, rb, n = w_next("winA4")
        raws = qk_wave_start(rt, rb)
        w_done(n)
        gk0 = off["gqk"] + 2 * l + 1
        def fin_k(g, bk, rs):
            nc.vector.scalar_tensor_tensor(out=SKT[0:64, g, 0, :], in0=bk[0:64, :], scalar=PAR[0:64, gk0:gk0 + 1], in1=rs[0:64, :], op0=ALU.mult, op1=ALU.mult)
            return nc.vector.scalar_tensor_tensor(out=SKT[64:128, g, 1, :], in0=bk[64:128, :], scalar=PAR[64:128, gk0:gk0 + 1], in1=rs[64:128, :], op0=ALU.mult, op1=ALU.mult)
        qk_wave_finish(raws, fin_k, SKTb)

        cxs = [mixer_stage1(ti, l, b) for b in range(NBLK)]
        for b in range(NBLK):
            mixer_stage2(ti, l, b, cxs[b])
        for b in range(NBLK):
            mixer_stage2b(ti, l, b, cxs[b])
            if b > 0:
                mixer_stage2d(ti, l, b - 1, cxs[b - 1])
        mixer_stage2d(ti, l, NBLK - 1, cxs[NBLK - 1])

        S.act(lambda: nc.scalar.copy(out=SKP[:, l], in_=SKT[:, :, :, (NBLK - 1) * 128:NBLK * 128]), reads=[SKTb], writes=[SKPb[l]])
        S.act(lambda: nc.scalar.copy(out=SVP[:, l], in_=SVA[:, NBLK - 1]), reads=[SVAb[NBLK - 1]], writes=[SVPb[l]])

        for c in range(8):
            rt, rb, n = w_next(f"mix{c}")
            bga, bgab = nb(); bya, byab = nb(); bgb_, bgbb = nb(); byb, bybb = nb()
            def mm8(bk, o, rt=rt):
                def f_():
                    for kc in range(KC):
                        ins = nc.tensor.matmul(bk[:], lhsT=rt[:, o + kc * 128:o + (kc + 1) * 128], rhs=XN[:, kc, :], start=(kc == 0), stop=(kc == KC - 1))
                    return ins
                return f_
            def mm4(bk, o, src, rt=rt):
                def f_():
                    for kc in range(4):
                        ins = nc.tensor.matmul(bk[:], lhsT=rt[:, o + kc * 128:o + (kc + 1) * 128], rhs=src(kc), start=(kc == 0), stop=(kc == 3))
                    return ins
                return f_
            S.pe(mm8(bga, 0), reads=[rb, XNb], writes=[bgab])
            S.pe(mm4(bya, 2048, lambda k: HAGT(k)), reads=[rb] + Hb[16:20], writes=[byab])
            S.pe(mm8(bgb_, 1024), reads=[rb, XNb], writes=[bgbb])
            S.pe(mm4(byb, 2560, lambda k: HBT[:, k, :]), reads=[rb, HBTb], writes=[bybb])
            w_done(n)
            sa, sab = tmpA.get()
            S.act(lambda sa=sa, bga=bga: nc.scalar.activation(out=sa[:], in_=bga[:], func=AF.Sigmoid), reads=[bgab], writes=[sab])
            sb_, sbb = tmpA.get()
            S.act(lambda sb_=sb_, bgb_=bgb_: nc.scalar.activation(out=sb_[:], in_=bgb_[:], func=AF.Sigmoid), reads=[bgbb], writes=[sbb])
            m1, m1b = tmpB.get()
            S.dve(lambda m1=m1, bya=bya, sa=sa: nc.vector.tensor_tensor(out=m1[:], in0=bya[:], in1=sa[:], op=ALU.mult), reads=[byab, sab], writes=[m1b])
            m2, m2b = tmpB.get()
            S.dve(lambda m2=m2, byb=byb, sb_=sb_: nc.vector.tensor_tensor(out=m2[:], in0=byb[:], in1=sb_[:], op=ALU.mult), reads=[bybb, sbb], writes=[m2b])
            S.dve(lambda c=c, m1=m1, m2=m2: nc.vector.tensor_tensor(out=MIXT(c), in0=m1[:], in1=m2[:], op=ALU.add), reads=[m1b, m2b], writes=[Hb[8 + c]])
            fb(bgab, byab, bgbb, bybb)
        for c in range(8):
            rt, rb, n = w_next(f"wout{c}")
            if c == 0:
                ns = norm_begin((l, 2))
            bk, bb = nb()
            def mmw(rt=rt, bk=bk):
                for kc in range(KC):
                    ins = nc.tensor.matmul(bk[:], lhsT=rt[:, kc * 128:(kc + 1) * 128], rhs=MIXT(kc), start=(kc == 0), stop=(kc == KC - 1))
                return ins
            S.pe(mmw, reads=[rb] + Hb[8:16], writes=[bb])
            w_done(n)
            S.dve(lambda c=c, bk=bk: nc.vector.tensor_tensor(out=XT[:, c, :], in0=bk[:], in1=XT[:, c, :], op=ALU.add), reads=[bb, XTb[c]], writes=[XTb[c]])
            fb(bb)
            norm_feed(ns, c)
        return ns

    def mixer_stage1(ti, l, b):
        gb = ti * NBLK + b
        bc = slice(b * 128, (b + 1) * 128)
        bmisc, bmiscb = nb(); bmv, bmvb = nb(); bmo, bmob = nb()
        def mmB(bk, c0, n_):
            def f_():
                for kc in range(KC):
                    ins = nc.tensor.matmul(bk[:, 0:n_], lhsT=XN[:, kc, bc], rhs=WB[:, kc, c0:c0 + n_], start=(kc == 0), stop=(kc == KC - 1))
                return ins
            return f_
        S.pe(mmB(bmisc, 1024, 392), reads=[XNb, WBb], writes=[bmiscb])
        S.pe(mmB(bmv, 0, 512), reads=[XNb, WBb], writes=[bmvb])
        S.pe(mmB(bmo, 512, 512), reads=[XNb, WBb], writes=[bmob])
        sm = lambda: SM.get()
        gt, gtb = sm()
        g0 = off["bg"] + l * 8
        S.dve(lambda: nc.vector.tensor_tensor(out=gt[:, 0:8], in0=bmisc[:, 256:264], in1=PAR[:, g0:g0 + 8], op=ALU.add), reads=[bmiscb, PARb], writes=[gtb])
        e1, e1b = sm()
        S.act(lambda: nc.scalar.activation(out=e1[:, 0:4], in_=gt[:, 4:8], func=AF.Exp, scale=-1.0), reads=[gtb], writes=[e1b])
        l1, l1b = sm()
        S.act(lambda: nc.scalar.activation(out=l1[:, 0:4], in_=e1[:, 0:4], func=AF.Ln, bias=ONE_AP[:]), reads=[e1b, CONb], writes=[l1b])
        mk, mkb = MKK.get()
        S.dve(lambda: nc.vector.tensor_copy(out=mk[:], in_=bmisc[:, 0:256]), reads=[bmiscb], writes=[mkb])
        S.act(lambda: nc.scalar.copy(out=SVA[:, b, :, 0:64], in_=bmisc[:, 264:392].rearrange("p (g d) -> p g d", g=2)), reads=[bmiscb], writes=[SVAb[b]])
        mvs, mvsb = MVs.get()
        S.act(lambda: nc.scalar.copy(out=mvs[:], in_=bmv[:]), reads=[bmvb], writes=[mvsb])
        smo, smob = SMO.get()
        S.act(lambda: nc.scalar.activation(out=smo[:], in_=bmo[:], func=AF.Sigmoid), reads=[bmob], writes=[smob])
        fb(bmiscb, bmvb, bmob)
        bgt, bgtb = nb()
        def mmg():
            nc.tensor.matmul(bgt[:, 0:4], lhsT=tri4[:, 0:128], rhs=l1[:, 0:4], start=True, stop=True)
            return nc.tensor.matmul(bgt[:, 8:12], lhsT=onesf[:], rhs=l1[:, 0:4], start=True, stop=True)
        S.pe(mmg, reads=[l1b, CONb], writes=[bgtb])
        a1, a1b = sm()
        S.dve(lambda: nc.vector.tensor_tensor(out=a1[:, 0:4], in0=bgt[:, 0:4], in1=gt[:, 0:4], op=ALU.add), reads=[bgtb, gtb], writes=[a1b])
        cc, ccb = sm()
        S.act(lambda: nc.scalar.activation(out=cc[:, 0:4], in_=a1[:, 0:4], func=AF.Exp), reads=[a1b], writes=[ccb])
        fl, flb = sm()
        S.act(lambda: nc.scalar.activation(out=fl[:, 0:4], in_=bgt[:, 0:4], func=AF.Exp), reads=[bgtb], writes=[flb])
        eg, egb = sm()
        def feg():
            nc.scalar.activation(out=eg[0:64, 0:2], in_=bgt[0:64, 8:12:2], func=AF.Exp, scale=-1.0)
            return nc.scalar.activation(out=eg[64:128, 0:2], in_=bgt[64:128, 9:13:2], func=AF.Exp, scale=-1.0)
        S.act(feg, reads=[bgtb], writes=[egb])
        fb(bgtb)
        vpp, vppb = VPP.get()
        def fv():
            for h in range(4):
                nc.vector.tensor_scalar(out=vpp[:, h, 0:128], in0=mvs[:, h * 128:(h + 1) * 128], scalar1=cc[:, h:h + 1], scalar2=None, op0=ALU.mult)
            return nc.vector.tensor_copy(out=vpp[:, :, 128], in_=cc[:, 0:4])
        S.dve(fv, reads=[mvsb, ccb], writes=[vppb])
        gs, gsb = GSP.get()
        m0 = off["mlg"] + l * 512
        S.dve(lambda: nc.vector.tensor_tensor(out=gs[:], in0=smo[:], in1=PAR[:, m0:m0 + 512], op=ALU.mult), reads=[smob, PARb], writes=[gsb])

        return dict(cc=cc, ccb=ccb, fl=fl, flb=flb, eg=eg, egb=egb, vpp=vpp, vppb=vppb, gs=gs, gsb=gsb, mk=mk, mkb=mkb)

    def mixer_stage2(ti, l, b, cx):
        gb = ti * NBLK + b
        bc = slice(b * 128, (b + 1) * 128)
        sm = lambda: SM.get()
        bst, bstb = nb()
        def mms():
            for h in range(4):
                hp = (h % 2) * 64
                ins = nc.tensor.matmul(bst[:, h * 128:(h + 1) * 128], lhsT=MKT[:, h // 2, bc], rhs=MQT[:, h, bc], start=True, stop=True)
            return ins
        S.pe(mms, reads=[MKTb, MQTb], writes=[bstb])
        ptm, ptmb = PTM.get()
        S.dve(lambda: nc.vector.tensor_tensor(out=ptm[:], in0=bst[:], in1=tri4[:], op=ALU.mult), reads=[bstb, CONb], writes=[ptmb])
        fb(bstb)
        cx["ptm"] = ptm
        cx["ptmb"] = ptmb

    def mixer_stage2b(ti, l, b, cx):
        gb = ti * NBLK + b
        bc = slice(b * 128, (b + 1) * 128)
        sm = lambda: SM.get()
        fl, flb, eg, egb, vpp, vppb, gs, gsb, mk, mkb, ptm, ptmb = (cx[k] for k in ("fl", "flb", "eg", "egb", "vpp", "vppb", "gs", "gsb", "mk", "mkb", "ptm", "ptmb"))
        kbs = [1] if gb == 0 else [0, 1]
        pts = {}
        for g in range(2):
            for kb in kbs:
                bss, bssb = nb()
                def mmss(g=g, kb=kb, bss=bss):
                    for i in range(4):
                        hq = 4 * g + i
                        hf = hq % 2
                        if kb == 1:
                            kt = SKT[:, g, hf, bc]
                        elif b > 0:
                            kt = SKT[:, g, hf, (b - 1) * 128:b * 128]
                        else:
                            kt = SKP[:, l, g, hf, :]
                        ins = nc.tensor.matmul(bss[:, i * 128:(i + 1) * 128], lhsT=kt, rhs=SQT[:, hq // 2, bc], start=True, stop=True)
                    return ins
                S.pe(mmss, reads=[SKTb, SKPb[l], SQTb], writes=[bssb])
                ee, eeb = tmpA.get()
                S.act(lambda ee=ee, bss=bss: nc.scalar.activation(out=ee[:], in_=bss[:], func=AF.Exp), reads=[bssb], writes=[eeb])
                fb(bssb)
                pt, ptb = PTS.get()
                S.dve(lambda pt=pt, ee=ee, g=g, kb=kb: nc.vector.tensor_tensor(out=pt[:], in0=ee[:], in1=EB[:, kb, 4 * g:4 * g + 4, :].rearrange("p h t -> p (h t)"), op=ALU.mult),
                      reads=[eeb, CONb], writes=[ptb])
                pts[(g, kb)] = (pt, ptb)
        bn = [nb(), nb()]
        def mmn():
            for h in range(4):
                hp = (h % 2) * 64
                o = bn[h // 2][0][:, (h % 2) * 129:(h % 2) * 129 + 129]
                nc.tensor.matmul(o, lhsT=ptm[:, h * 128:(h + 1) * 128], rhs=vpp[:, h, :], start=True, stop=False)
                ins = nc.tensor.matmul(o, lhsT=MQT[:, h, bc], rhs=CSB[:, l, h // 2, :], start=False, stop=True)
            return ins
        S.pe(mmn, reads=[ptmb, vppb, MQTb, CSBb[l]], writes=[bn[0][1], bn[1][1]])
        bu = [nb(), nb()]
        def mmu():
            for i in range(2):
                ins = nc.tensor.matmul(bu[i][0][:, 0:258], lhsT=mk[:, i * 128:(i + 1) * 128], rhs=vpp[:, 2 * i:2 * i + 2, :].rearrange("p a b -> p (a b)"), start=True, stop=True)
            return ins
        S.pe(mmu, reads=[mkb, vppb], writes=[bu[0][1], bu[1][1]])
        for i in range(2):
            def fadd(i=i):
                nc.vector.tensor_tensor(out=CS[0:64, l, i, :], in0=bu[i][0][0:64, 0:129], in1=CS[0:64, l, i, :], op=ALU.add)
                return nc.vector.tensor_tensor(out=CS[64:128, l, i, :], in0=bu[i][0][64:128, 129:258], in1=CS[64:128, l, i, :], op=ALU.add)
            S.dve(fadd, reads=[bu[i][1], CSb[l]], writes=[CSb[l]])
            S.dve(lambda i=i: nc.vector.tensor_scalar(out=CS[:, l, i, :], in0=CS[:, l, i, :], scalar1=eg[:, i:i + 1], scalar2=None, op0=ALU.mult),
                  reads=[egb, CSb[l]], writes=[CSb[l]])
        fb(bu[0][1], bu[1][1])
        S.act(lambda: nc.scalar.copy(out=CSB[:, l], in_=CS[:, l]), reads=[CSb[l]], writes=[CSBb[l]])
        dn, dnb = sm()
        ad, adb = sm()
        def fad():
            for i in range(2):
                ins = nc.scalar.activation(out=ad[:, 2 * i:2 * i + 2], in_=bn[i][0][:, 128:258:129], func=AF.Abs)
            return ins
        S.act(fad, reads=[bn[0][1], bn[1][1]], writes=[adb])
        S.dve(lambda: nc.vector.tensor_tensor(out=dn[:, 0:4], in0=ad[:, 0:4], in1=fl[:, 0:4], op=ALU.max), reads=[adb, flb], writes=[dnb])
        rdn, rdnb = sm()
        S.dve(lambda: nc.vector.reciprocal(out=rdn[:, 0:4], in_=dn[:, 0:4]), reads=[dnb], writes=[rdnb])
        nss, nssb = sm()
        junk, junkb = tmpH.get()
        def fss():
            for h in range(4):
                o = (h % 2) * 129
                ins = nc.scalar.activation(out=junk[:, h * 128:(h + 1) * 128], in_=bn[h // 2][0][:, o:o + 128], func=AF.Square, accum_out=nss[:, h:h + 1])
            return ins
        S.act(fss, reads=[bn[0][1], bn[1][1]], writes=[nssb, junkb])
        t2, t2b = sm()
        S.dve(lambda: nc.vector.tensor_tensor(out=t2[:, 0:4], in0=rdn[:, 0:4], in1=rdn[:, 0:4], op=ALU.mult), reads=[rdnb], writes=[t2b])
        t3, t3b = sm()
        S.dve(lambda: nc.vector.tensor_tensor(out=t3[:, 0:4], in0=t2[:, 0:4], in1=nss[:, 0:4], op=ALU.mult), reads=[t2b, nssb], writes=[t3b])
        ln2, ln2b = sm()
        S.act(lambda: nc.scalar.activation(out=ln2[:, 0:4], in_=t3[:, 0:4], func=AF.Ln, scale=1.0 / 128, bias=EPS_AP[:]), reads=[t3b, CONb], writes=[ln2b])
        rst, rstb = sm()
        S.act(lambda: nc.scalar.activation(out=rst[:, 0:4], in_=ln2[:, 0:4], func=AF.Exp, scale=-0.5), reads=[ln2b], writes=[rstb])
        fac, facb = sm()
        S.dve(lambda: nc.vector.tensor_tensor(out=fac[:, 0:4], in0=rdn[:, 0:4], in1=rst[:, 0:4], op=ALU.mult), reads=[rdnb, rstb], writes=[facb])
        htk, htkb = HTK.get()
        def fh():
            for h in range(4):
                o = (h % 2) * 129
                ins = nc.vector.scalar_tensor_tensor(out=htk[:, h * 128:(h + 1) * 128], in0=bn[h // 2][0][:, o:o + 128], scalar=fac[:, h:h + 1],
                                                     in1=gs[:, h * 128:(h + 1) * 128], op0=ALU.mult, op1=ALU.mult)
            return ins
        S.dve(fh, reads=[bn[0][1], bn[1][1], facb, gsb], writes=[htkb])
        fb(bn[0][1], bn[1][1])
        bo = [nb(), nb()]
        def mmo():
            for hq in range(8):
                g, i = hq // 4, hq % 4
                o = bo[g][0][:, i * 65:(i + 1) * 65]
                if 0 in kbs:
                    vprev = SVA[:, b - 1, g, :] if b > 0 else SVP[:, l, g, :]
                    nc.tensor.matmul(o, lhsT=pts[(g, 0)][0][:, i * 128:(i + 1) * 128], rhs=vprev, start=True, stop=False)
                ins = nc.tensor.matmul(o, lhsT=pts[(g, 1)][0][:, i * 128:(i + 1) * 128], rhs=SVA[:, b, g, :], start=(0 not in kbs), stop=True)
            return ins
        S.pe(mmo, reads=[v[1] for v in pts.values()] + [SVAb[b], SVAb[(b - 1) % NBLK], SVPb[l]], writes=[bo[0][1], bo[1][1]])
        d8, d8b = sm()
        def fd8():
            for g in range(2):
                ins = nc.vector.tensor_tensor(out=d8[:, 4 * g:4 * g + 4], in0=bo[g][0][:, 64:260:65], in1=ES[:, l * 8 + 4 * g:l * 8 + 4 * g + 4], op=ALU.add)
            return ins
        S.dve(fd8, reads=[bo[0][1], bo[1][1], CONb], writes=[d8b])
        r8, r8b = sm()
        S.dve(lambda: nc.vector.reciprocal(out=r8[:, 0:8], in_=d8[:, 0:8]), reads=[d8b], writes=[r8b])
        hbk, hbkb = HTK.get()
        def fhb():
            for hq in range(8):
                g, i = hq // 4, hq % 4
                ins = nc.vector.tensor_scalar(out=hbk[:, hq * 64:(hq + 1) * 64], in0=bo[g][0][:, i * 65:i * 65 + 64], scalar1=r8[:, hq:hq + 1], scalar2=None, op0=ALU.mult)
            return ins
        S.dve(fhb, reads=[bo[0][1], bo[1][1], r8b], writes=[hbkb])
        fb(bo[0][1], bo[1][1])
        cx['htk'] = htk; cx['htkb'] = htkb; cx['hbk'] = hbk; cx['hbkb'] = hbkb

    def mixer_stage2d(ti, l, b, cx):
        bc = slice(b * 128, (b + 1) * 128)
        htk, htkb, hbk, hbkb = cx['htk'], cx['htkb'], cx['hbk'], cx['hbkb']
        def ftr2():
            for k in range(4):
                ins = nc.tensor.transpose(bankT[:, 512 + k * 128:512 + (k + 1) * 128], hbk[:, k * 128:(k + 1) * 128], identb[:])
            return ins
        def ftr():
            for h in range(4):
                ins = nc.tensor.transpose(bankT[:, h * 128:(h + 1) * 128], htk[:, h * 128:(h + 1) * 128], identb[:])
            return ins
        S.pe(ftr, reads=[htkb, CONb], writes=[bankTb[0]])
        S.act(lambda: nc.scalar.copy(out=H[:, 16:20, bc], in_=bankT[:, 0:512].rearrange("p (h t) -> p h t", h=4)), reads=[bankTb[0]], writes=Hb[16:20])

        S.pe(ftr2, reads=[hbkb, CONb], writes=[bankTb[1]])
        S.dve(lambda: nc.vector.tensor_copy(out=HBT[:, :, bc], in_=bankT[:, 512:1024].rearrange("p (h t) -> p h t", h=4)), reads=[bankTb[1]], writes=[HBTb])

    def ple(ti, l, ns_in, want_next):
        for k in range(2):
            bk, bb = nb()
            def tr(k=k, bk=bk):
                for b in range(NBLK):
                    ins = nc.tensor.transpose(bk[:, b * 128:(b + 1) * 128], PS_[:, b, k * 128:(k + 1) * 128], identf[:])
                return ins
            S.pe(tr, reads=[PSb_, CONb], writes=[bb])
            S.act(lambda k=k, bk=bk: nc.scalar.copy(out=PT[:, k, :], in_=bk[:]), reads=[bb], writes=[PTb])
            fb(bb)
        dfr = norm_end(ns_in, l, 3)
        ns = norm_begin() if want_next else None
        for c in range(8):
            rt, rb, n = w_next(f"ple{c}")
            bg, bgb = nb(); bp, bpb = nb()
            def mmg(rt=rt, bg=bg):
                for kc in range(KC):
                    ins = nc.tensor.matmul(bg[:], lhsT=rt[:, kc * 128:(kc + 1) * 128], rhs=XN[:, kc, :], start=(kc == 0), stop=(kc == KC - 1))
                return ins
            def mmp(rt=rt, bp=bp):
                for k in range(2):
                    ins = nc.tensor.matmul(bp[:], lhsT=rt[:, 1024 + k * 128:1024 + (k + 1) * 128], rhs=PT[:, k, :], start=(k == 0), stop=(k == 1))
                return ins
            S.pe(mmg, reads=[rb, XNb], writes=[bgb])
            S.pe(mmp, reads=[rb, PTb], writes=[bpb])
            w_done(n)
            sg, sgb = tmpA.get()
            if dfr:
                gs_, gsb_ = tmpB.get()
                S.dve(lambda gs_=gs_, bg=bg: nc.vector.tensor_tensor(out=gs_[:], in0=bg[:], in1=RS[:], op=ALU.mult), reads=[bgb, RSb], writes=[gsb_])
                S.act(lambda sg=sg, gs_=gs_: nc.scalar.activation(out=sg[:], in_=gs_[:], func=AF.Sigmoid), reads=[gsb_], writes=[sgb])
            else:
                S.act(lambda sg=sg, bg=bg: nc.scalar.activation(out=sg[:], in_=bg[:], func=AF.Sigmoid), reads=[bgb], writes=[sgb])
            m, mb = tmpB.get()
            S.dve(lambda m=m, bp=bp, sg=sg: nc.vector.tensor_tensor(out=m[:], in0=bp[:], in1=sg[:], op=ALU.mult), reads=[bpb, sgb], writes=[mb])
            S.dve(lambda c=c, m=m: nc.vector.tensor_tensor(out=XT[:, c, :], in0=m[:], in1=XT[:, c, :], op=ALU.add), reads=[mb, XTb[c]], writes=[XTb[c]])
            fb(bgb, bpb)
            norm_feed(ns, c)
        return ns

    EPS_AP = sb("EPS_AP", [128, 1]); ONE_AP = sb("ONE_AP", [128, 1])
    S.dve(lambda: nc.vector.memset(EPS_AP[:], EPS), writes=[CONb])
    S.dve(lambda: nc.vector.memset(ONE_AP[:], 1.0), writes=[CONb])
    import os as _os
    KPH = _os.environ.get("KPH", "cfmp")
    for l in range(L):
        if "c" in KPH:
            emit_casts(l)
    setup()
    for ti in range(NT):
        ns = prologue(ti)
        for l in range(L):
            ns = ffn(l, 0, ns)
            ns = mixer(ti, l, ns)
            ns = ffn(l, 1, ns, ti)
            ns = ple(ti, l, ns, l < L - 1)
        epilogue(ti)
    S.final_wait("sp", XSl)
    print("sbuf bytes remaining", nc.sbuf_bytes_remaining)
    S.emit()
    return nc


_CACHE = {}


def kernel(x, p, ffn1_norm, ffn1_wi, ffn1_wo, mix_norm, w_in, b_igate, b_fgate, ml_out_norm,
           q_norm, k_norm, sinks, rel_bias, w_a, w_b, w_out, ffn2_norm, ffn2_wi, ffn2_wo,
           ple_norm, w_ple_gate, w_ple):
    f = lambda a: np.ascontiguousarray(np.asarray(a, dtype=np.float32))
    x = f(x); p = f(p)
    B, S_LEN, _ = x.shape
    L = p.shape[0]
    key = (S_LEN, L)
    if key not in _CACHE:
        _CACHE[key] = build_program(S_LEN, L)
    nc = _CACHE[key]
    params = pack_params(L, f(ffn1_norm), f(mix_norm), f(ffn2_norm), f(ple_norm), f(b_igate), f(b_fgate),
                         f(ml_out_norm), f(q_norm), f(k_norm), f(sinks))
    consts = make_consts(f(rel_bias))
    shared = {"ffn1_wi": f(ffn1_wi), "ffn2_wi": f(ffn2_wi), "ffn1_wo": f(ffn1_wo), "ffn2_wo": f(ffn2_wo),
              "w_in": f(w_in), "w_a": f(w_a), "w_b": f(w_b), "w_out": f(w_out), "w_ple_gate": f(w_ple_gate),
              "w_ple": f(w_ple), "params": params}
    shared.update(consts)
    in_maps = []
    for i in range(B):
        m = dict(shared)
        m["x"] = np.ascontiguousarray(x[i])
        m["p"] = np.ascontiguousarray(p[:, i])
        in_maps.append(m)
    res = run_bass_kernel_spmd(nc, in_maps, core_ids=list(range(B)))
    return np.stack([np.asarray(res.results[i]["out"], dtype=np.float32) for i in range(B)], axis=0)
```

```python
import numpy as np
import concourse.bass as bass
import concourse.mybir as mybir
from concourse.bass_utils import run_bass_kernel_spmd

F32 = mybir.dt.float32
BF16 = mybir.dt.bfloat16
AF = mybir.ActivationFunctionType
ALU = mybir.AluOpType

D = 1024
KC = 8
FF = 2816
NJ = 22
PD = 256
EPS = 1e-6
OFF_MQ, OFF_MK, OFF_MV, OFF_MO, OFF_MI, OFF_MF = 0, 256, 512, 1024, 1536, 1540
OFF_SQ, OFF_SK, OFF_SV, OFF_GA, OFF_GB = 1544, 2056, 2184, 2312, 3336
D_IN = 4360
NB_COLS = 1416
RING = 5
SLOT = 3072
T = 512
NBLK = T // 128


class Buf:
    __slots__ = ("name", "w", "r", "dsem", "dcount", "live")

    def __init__(self, name):
        self.name = name
        self.w = None
        self.r = {}
        self.dsem = None
        self.dcount = 0
        self.live = False

    def add_read(self, ev):
        s, v = ev
        if self.r.get(s, 0) < v:
            self.r[s] = v


class Sched:
    def __init__(self, nc):
        self.nc = nc
        self.sems = []
        self.engs = {}
        for name in ("pe", "act", "dve", "pool", "sp"):
            sid = self.new_sem("s_" + name)
            self.engs[name] = dict(sem=sid, count=0, waited={}, prog=[])

    def new_sem(self, name):
        self.sems.append(self.nc.alloc_semaphore(name))
        return len(self.sems) - 1

    def _deps(self, E, reads, writes):
        need = {}
        for b in reads:
            if b.w is not None and need.get(b.w[0], 0) < b.w[1]:
                need[b.w[0]] = b.w[1]
        for b in writes:
            if b.w is not None and need.get(b.w[0], 0) < b.w[1]:
                need[b.w[0]] = b.w[1]
            for s, v in b.r.items():
                if need.get(s, 0) < v:
                    need[s] = v
        waits = []
        for s, v in need.items():
            if E["waited"].get(s, 0) >= v:
                continue
            E["waited"][s] = v
            waits.append((s, v))
        return waits

    def op(self, eng, fn, reads=(), writes=()):
        E = self.engs[eng]
        waits = self._deps(E, reads, writes)
        E["count"] += 1
        ev = (E["sem"], E["count"])
        E["prog"].append((waits, fn, (E["sem"], 1)))
        for b in reads:
            b.add_read(ev)
        for b in writes:
            b.w = ev
            b.r = {}

    def pe(self, fn, reads=(), writes=()):
        self.op("pe", fn, reads, writes)

    def act(self, fn, reads=(), writes=()):
        self.op("act", fn, reads, writes)

    def dve(self, fn, reads=(), writes=()):
        self.op("dve", fn, reads, writes)

    def dma(self, queue, fn, owner, reads=(), writes=(), track=()):
        E = self.engs[queue]
        waits = self._deps(E, reads, writes)
        if owner.dsem is None:
            owner.dsem = self.new_sem("d_" + owner.name)
        owner.dcount += 16
        ev = (owner.dsem, owner.dcount)
        E["prog"].append((waits, fn, (owner.dsem, 16)))
        for b in reads:
            b.add_read(ev)
        for b in writes:
            b.w = ev
            b.r = {}
        for b in track:
            b.w = ev

    def final_wait(self, eng, bufs):
        E = self.engs[eng]
        waits = self._deps(E, bufs, bufs)
        E["prog"].append((waits, None, None))

    def emit(self):
        nc = self.nc
        sems = self.sems
        with nc.Block() as block:
            def run(name):
                def body(e):
                    for waits, fn, inc in self.engs[name]["prog"]:
                        for s, v in waits:
                            e.wait_ge(sems[s], v)
                        if fn is not None:
                            ins = fn()
                            ins.then_inc(sems[inc[0]], inc[1])
                return body
            block.tensor(run("pe"))
            block.scalar(run("act"))
            block.vector(run("dve"))
            block.gpsimd(run("pool"))
            block.sync(run("sp"))


def param_layout(L):
    off = {}
    o = 0
    off["ng"] = o; o += L * 4 * 8
    off["bg"] = o; o += L * 8
    off["mlg"] = o; o += L * 512
    off["gqk"] = o; o += L * 2
    off["snk"] = o; o += L * 8
    off["n"] = o
    return off


def pack_params(L, ffn1_norm, mix_norm, ffn2_norm, ple_norm, b_igate, b_fgate, ml_out_norm,
                q_norm, k_norm, sinks):
    off = param_layout(L)
    P = np.zeros((128, off["n"]), np.float32)
    ng = np.stack([ffn1_norm, mix_norm, ffn2_norm, ple_norm], axis=1)
    ng = ng.reshape(L, 4, 8, 128).transpose(3, 0, 1, 2).reshape(128, -1)
    P[:, off["ng"]:off["ng"] + L * 32] = ng
    bg = np.concatenate([b_igate, b_fgate], axis=1).reshape(1, -1)
    P[:, off["bg"]:off["bg"] + L * 8] = np.broadcast_to(bg, (128, L * 8))
    P[:, off["mlg"]:off["mlg"] + L * 512] = np.broadcast_to(ml_out_norm.reshape(1, -1), (128, L * 512))
    gqk = np.stack([q_norm, k_norm], axis=1)
    gqk = np.concatenate([gqk, gqk], axis=2).transpose(2, 0, 1).reshape(128, -1)
    P[:, off["gqk"]:off["gqk"] + L * 2] = gqk
    P[:, off["snk"]:off["snk"] + L * 8] = np.broadcast_to(sinks.reshape(1, -1), (128, L * 8))
    return P


def t5_bucket(dist):
    max_exact = 16
    d = np.maximum(dist, 0)
    large = max_exact + (np.log(np.maximum(d, 1) / max_exact) / np.log(128 / max_exact)
                         * (32 - max_exact)).astype(np.int32)
    large = np.minimum(large, 31)
    return np.where(d < max_exact, d, large).astype(np.int32)


def make_consts(rel_bias):
    s = np.arange(128)[:, None]
    t = np.arange(128)[None, :]
    tri = (s <= t).astype(np.float32)
    c = {}
    c["ident"] = np.eye(128, dtype=np.float32)
    c["tri4"] = np.tile(tri, (1, 4))
    bo = np.zeros((128, 128), np.float32)
    bo[:64, :64] = 1.0
    bo[64:, 64:] = 1.0
    c["blockones"] = bo
    dist_prev = t + 128 - s
    dist_cur = t - s
    valid = np.stack([(s > t), (s <= t)]).astype(np.float32)
    idx = np.stack([t5_bucket(np.clip(dist_prev, 0, 127)), t5_bucket(np.clip(dist_cur, 0, 127))])
    g = np.asarray(rel_bias, np.float32)[idx]
    c["biasg"] = np.ascontiguousarray(g.transpose(1, 0, 3, 2)).reshape(128, 2 * 8 * 128)
    c["maskc"] = np.ascontiguousarray(np.broadcast_to(valid.transpose(1, 0, 2)[:, :, None, :],
                                                      (128, 2, 8, 128))).reshape(128, 2 * 8 * 128)
    return c


def build_program(S_LEN, L):
    NT = S_LEN // T
    nc = bass.Bass("TRN2", target_bir_lowering=False)
    S = Sched(nc)
    off = param_layout(L)

    def dram_in(name, shape, dt=F32):
        return nc.dram_tensor(name, list(shape), dt, kind="ExternalInput").ap()

    x_d = dram_in("x", [S_LEN, D])
    p_d = dram_in("p", [L, S_LEN, PD])
    out_d = nc.dram_tensor("out", [S_LEN, D], F32, kind="ExternalOutput").ap()
    wi_d = [dram_in("ffn1_wi", [L, D, 2 * FF]), dram_in("ffn2_wi", [L, D, 2 * FF])]
    wo_d = [dram_in("ffn1_wo", [L, FF, D]), dram_in("ffn2_wo", [L, FF, D])]
    win_d = dram_in("w_in", [L, D, D_IN])
    wa_d = dram_in("w_a", [L, 512, D])
    wb_d = dram_in("w_b", [L, 512, D])
    wout_d = dram_in("w_out", [L, D, D])
    wpg_d = dram_in("w_ple_gate", [L, D, D])
    wple_d = dram_in("w_ple", [L, PD, D])
    par_d = dram_in("params", [128, off["n"]])
    ident_d = dram_in("ident", [128, 128])
    tri4_d = dram_in("tri4", [128, 512])
    bo_d = dram_in("blockones", [128, 128])
    biasg_d = dram_in("biasg", [128, 2048])
    maskc_d = dram_in("maskc", [128, 2048])

    def scr(name, shape):
        return nc.dram_tensor(name, list(shape), BF16).ap()

    sc = []
    for l in range(L):
        e = {}
        e["wi"] = [scr(f"sc_wi{f}_{l}", [NJ, 128, 2048]) for f in range(2)]
        e["wo"] = [scr(f"sc_wo{f}_{l}", [8, 128, NJ * 128]) for f in range(2)]
        e["winA"] = scr(f"sc_winA_{l}", [5, 128, 2048])
        e["mix"] = scr(f"sc_mix_{l}", [8, 128, 3072])
        e["wout"] = scr(f"sc_wout_{l}", [8, 128, 1024])
        e["ple"] = scr(f"sc_ple_{l}", [8, 128, 1280])
        e["winB"] = scr(f"sc_winB_{l}", [128, 8 * NB_COLS])
        e["b"] = {k: Buf(f"b_{k}_{l}") for k in ("wi0", "wi1", "wo0", "wo1", "winA", "mix", "wout", "ple", "winB")}
        sc.append(e)

    def cast(dst, src, owner):
        S.dma("pool", lambda: nc.gpsimd.dma_start(out=dst, in_=src), owner, track=[owner])

    def kview(w2d, c0, n):
        return w2d.rearrange("(k p) c -> p k c", p=128)[:, :, c0:c0 + n]

    def emit_casts(l):
        e = sc[l]
        for f in range(2):
            for j in range(NJ):
                rec = e["wi"][f][j].rearrange("p (k h n) -> p k h n", k=8, h=2)
                for h in range(2):
                    cast(rec[:, :, h, :], kview(wi_d[f][l], h * FF + j * 128, 128), e["b"][f"wi{f}"])
            for c in range(8):
                rec = e["wo"][f][c].rearrange("p (j n) -> p j n", n=128)
                cast(rec, kview(wo_d[f][l], c * 128, 128), e["b"][f"wo{f}"])
            if f == 0:
                emit_casts_mixer(l)
        for c in range(8):
            rec = e["ple"][c]
            cast(rec[:, 0:1024].rearrange("p (k n) -> p k n", n=128), kview(wpg_d[l], c * 128, 128), e["b"]["ple"])
            cast(rec[:, 1024:1280].rearrange("p (k n) -> p k n", n=128), kview(wple_d[l], c * 128, 128), e["b"]["ple"])

    def emit_casts_mixer(l):
        e = sc[l]
        w = win_d[l]
        rb = e["winB"].rearrange("p (k c) -> p k c", c=NB_COLS)
        for (d0, s0, n) in ((0, OFF_MV, 512), (512, OFF_MO, 512), (1024, OFF_MK, 256), (1280, OFF_MI, 8), (1288, OFF_SV, 128)):
            cast(rb[:, :, d0:d0 + n], kview(w, s0, n), e["b"]["winB"])
        recs = e["winA"]
        def ra(r, i):
            return recs[r].rearrange("p (i k n) -> p i k n", i=2, k=8)[:, i]
        cast(ra(0, 0), kview(w, OFF_SQ, 128), e["b"]["winA"])
        cast(ra(0, 1), kview(w, OFF_SQ + 128, 128), e["b"]["winA"])
        cast(ra(1, 0), kview(w, OFF_MQ, 128), e["b"]["winA"])
        cast(ra(1, 1), kview(w, OFF_MQ + 128, 128), e["b"]["winA"])
        cast(ra(2, 0), kview(w, OFF_SQ + 256, 128), e["b"]["winA"])
        cast(ra(2, 1), kview(w, OFF_SQ + 384, 128), e["b"]["winA"])
        cast(ra(3, 0), kview(w, OFF_MK, 128), e["b"]["winA"])
        cast(ra(3, 1), kview(w, OFF_MK + 128, 128), e["b"]["winA"])
        for g in range(2):
            for half in range(2):
                cast(ra(4, g)[:, :, half * 64:(half + 1) * 64], kview(w, OFF_SK + g * 64, 64), e["b"]["winA"])
        for c in range(8):
            rec = e["mix"][c]
            cast(rec[:, 0:1024].rearrange("p (k n) -> p k n", n=128), kview(w, OFF_GA + c * 128, 128), e["b"]["mix"])
            cast(rec[:, 1024:2048].rearrange("p (k n) -> p k n", n=128), kview(w, OFF_GB + c * 128, 128), e["b"]["mix"])
            cast(rec[:, 2048:2560].rearrange("p (k n) -> p k n", n=128), kview(wa_d[l], c * 128, 128), e["b"]["mix"])
            cast(rec[:, 2560:3072].rearrange("p (k n) -> p k n", n=128), kview(wb_d[l], c * 128, 128), e["b"]["mix"])
        for c in range(8):
            cast(e["wout"][c].rearrange("p (k n) -> p k n", n=128), kview(wout_d[l], c * 128, 128), e["b"]["wout"])

    def sb(name, shape, dt=F32):
        return nc.alloc_sbuf_tensor("sb_" + name, list(shape), dt)

    XT = sb("XT", [128, KC, T]); XTb = [Buf(f"XT{k}") for k in range(KC)]
    XN = sb("XN", [128, KC, T], BF16); XNb = Buf("XN")
    H = sb("H", [128, NJ, T], BF16); Hb = [Buf(f"H{j}") for j in range(NJ)]
    XSown = Buf("XS")
    ring = [sb(f"ring{i}", [128, SLOT], BF16) for i in range(RING)]
    ringb = [Buf(f"ring{i}") for i in range(RING)]
    WB = sb("WB", [128, KC, NB_COLS], BF16); WBb = Buf("WB")
    PAR = sb("PAR", [128, off["n"]]); PARb = Buf("PAR")
    identf = sb("identf", [128, 128]); identb = sb("identb", [128, 128], BF16)
    tri4 = sb("tri4", [128, 512])
    onesb = sb("onesb", [128, 128], BF16); onesf = sb("onesf", [128, 128])
    blockones = sb("blockones", [128, 128], BF16)
    EB = sb("EB", [128, 2, 8, 128], BF16)
    CONb = Buf("consts")
    GQ8 = sb("GQ8", [128, L]); ES = sb("ES", [128, L * 8])
    MQT = sb("MQTz", [128, 4, T], BF16); MQTb = Buf("MQT")
    MKT = sb("MKT", [128, 2, T], BF16); MKTb = Buf("MKT")
    SQT = sb("SQT", [128, 4, T], BF16); SQTb = Buf("SQT")
    SKT = sb("SKTz", [128, 2, 2, T], BF16); SKTb = Buf("SKT")
    SKP = sb("SKP", [128, L, 2, 2, 128], BF16); SKPb = [Buf(f"SKP{l}") for l in range(L)]
    SVA = sb("SVA", [128, NBLK, 2, 65], BF16); SVAb = [Buf(f"SVA{b}") for b in range(NBLK)]
    SVP = sb("SVP", [128, L, 2, 65], BF16); SVPb = [Buf(f"SVP{l}") for l in range(L)]
    HBT = sb("HBT", [128, 4, T], BF16); HBTb = Buf("HBT")
    CS = sb("CS", [128, L, 2, 129]); CSb = [Buf(f"CS{l}") for l in range(L)]
    CSB = sb("CSB", [128, L, 2, 129], BF16); CSBb = [Buf(f"CSB{l}") for l in range(L)]
    PS_ = sb("PS_", [128, NBLK, PD]); PSb_ = Buf("PS_")
    PT = sb("PT", [128, 2, T], BF16); PTb = Buf("PT")
    XS = H[:].rearrange("p j t -> p (j t)").bitcast(F32)[:, 0:NBLK * D].rearrange("p (b d) -> p b d", b=NBLK)
    XSl = Hb[0:16]
    MIXT = lambda c: H[:, 8 + c, :]
    HAGT = lambda k: H[:, 16 + k, :]

    class Pool2:
        def __init__(self, name, n, shape, dt=F32):
            self.t = [sb(f"{name}{i}", shape, dt) for i in range(n)]
            self.b = [Buf(f"{name}{i}") for i in range(n)]
            self.i = 0
        def get(self):
            i = self.i
            self.i = (i + 1) % len(self.t)
            return self.t[i], self.b[i]

    tmpA = Pool2("tmpA", 3, [128, T])
    tmpB = Pool2("tmpB", 3, [128, T])
    tmpH = Pool2("tmpH", 4, [128, T], BF16)
    MVs = Pool2("MVs", 2, [128, 512])
    SMO = Pool2("SMO", 2, [128, 512])
    MKK = Pool2("MKK", 4, [128, 256], BF16)
    VPP = Pool2("VPP", 4, [128, 4, 129], BF16)
    GSP = Pool2("GSP", 4, [128, 512])
    PTM = Pool2("PTM", 4, [128, 512], BF16)
    PTS = Pool2("PTS", 4, [128, 512], BF16)
    HTK = Pool2("HTK", 4, [128, 512], BF16)
    SM = Pool2("SM", 48, [128, 16])

    banks = [nc.alloc_psum_tensor(f"bank{i}", [128, 512], F32) for i in range(7)]
    bankT = nc.alloc_psum_tensor("bankT", [128, 1024], BF16)
    bankb = [Buf(f"bank{i}") for i in range(7)]
    _bt = Buf("bankT"); bankTb = [_bt, _bt]
    bstate = {"i": 0}

    def nb():
        for _ in range(7):
            i = bstate["i"]
            bstate["i"] = (i + 1) % 7
            if not bankb[i].live:
                bankb[i].live = True
                return banks[i], bankb[i]
        raise RuntimeError("no free PSUM bank")

    def fb(*bs):
        for b in bs:
            b.live = False

    plan = []
    for ti in range(NT):
        for l in range(L):
            for f in range(2):
                if f == 1:
                    for r in range(5):
                        plan.append((f"winA{r}", sc[l]["winA"][r], 2048, sc[l]["b"]["winA"]))
                    for c in range(8):
                        plan.append((f"mix{c}", sc[l]["mix"][c], 3072, sc[l]["b"]["mix"]))
                    for c in range(8):
                        plan.append((f"wout{c}", sc[l]["wout"][c], 1024, sc[l]["b"]["wout"]))
                for j in range(NJ):
                    plan.append((f"wi{f}_{j}", sc[l]["wi"][f][j], 2048, sc[l]["b"][f"wi{f}"]))
                for c in range(8):
                    plan.append((f"wo{f}_{c}", sc[l]["wo"][f][c], NJ * 128, sc[l]["b"][f"wo{f}"]))
            for c in range(8):
                plan.append((f"ple{c}", sc[l]["ple"][c], 1280, sc[l]["b"]["ple"]))
    import os as _os2
    _kph = _os2.environ.get("KPH", "cfmp")
    def _keep(k):
        if k[:2] in ("wi", "wo") and k[2] in "01" and k[3] == "_":
            return "f" in _kph
        if k.startswith("ple"):
            return "p" in _kph
        return "m" in _kph
    plan = [e_ for e_ in plan if _keep(e_[0])]
    wst = {"pos": 0, "loaded": 0}

    def w_load_upto(n):
        while wst["loaded"] <= n and wst["loaded"] < len(plan):
            m = wst["loaded"]
            key, src, ne, sbuf_ = plan[m]
            slot = m % RING
            S.dma("sp", lambda slot=slot, src=src, ne=ne: nc.sync.dma_start(out=ring[slot][:, 0:ne], in_=src),
                  ringb[slot], reads=[sbuf_], writes=[ringb[slot]])
            wst["loaded"] += 1

    def w_next(key):
        n = wst["pos"]
        assert plan[n][0] == key, (plan[n][0], key)
        w_load_upto(n)
        wst["pos"] += 1
        return ring[n % RING], ringb[n % RING], n

    def w_done(n):
        w_load_upto(n + RING)

    def setup():
        stg = XS[:, 0:2, :].rearrange("p a d -> p (a d)")
        stg2 = XS[:, 2:4, :].rearrange("p a d -> p (a d)")
        def ld(dst, src, owner):
            ws = XSl if owner is XSown else [owner]
            S.dma("sp", lambda: nc.sync.dma_start(out=dst, in_=src), owner, writes=ws)
        stb = stb2 = XSown
        ld(PAR[:], par_d, PARb)
        ld(identf[:], ident_d, CONb)
        ld(tri4[:], tri4_d, CONb)
        ld(stg[:, 0:128], bo_d, stb)
        ld(stg2, maskc_d, stb2)
        S.dve(lambda: nc.vector.tensor_copy(out=identb[:], in_=identf[:]), reads=[CONb], writes=[CONb])
        S.dve(lambda: nc.vector.tensor_copy(out=blockones[:], in_=stg[:, 0:128]), reads=XSl, writes=[CONb])
        S.dve(lambda: nc.vector.memset(onesb[:], 1.0), writes=[CONb])
        S.dve(lambda: nc.vector.memset(onesf[:], 1.0), writes=[CONb])
        ld(stg, biasg_d, stb)
        S.act(lambda: nc.scalar.activation(out=stg, in_=stg, func=AF.Exp), reads=XSl, writes=XSl)
        S.dve(lambda: nc.vector.tensor_tensor(out=EB[:].rearrange("p a h t -> p (a h t)"), in0=stg, in1=stg2, op=ALU.mult),
              reads=XSl, writes=[CONb])
        S.dve(lambda: nc.vector.tensor_scalar(out=GQ8[:], in0=PAR[:, off["gqk"]:off["gqk"] + 2 * L:2], scalar1=0.125, scalar2=None, op0=ALU.mult),
              reads=[PARb], writes=[CONb])
        S.act(lambda: nc.scalar.activation(out=ES[:], in_=PAR[:, off["snk"]:off["snk"] + 8 * L], func=AF.Exp), reads=[PARb], writes=[CONb])
        S.dve(lambda: nc.vector.memset(CS[:], 0.0), writes=CSb)
        S.dve(lambda: nc.vector.memset(CSB[:], 0.0), writes=CSBb)
        S.dve(lambda: nc.vector.memset(SVA[:], 1.0), writes=SVAb)
        S.dve(lambda: nc.vector.memset(SVP[:], 1.0), writes=SVPb)
        S.dve(lambda: nc.vector.memset(SKP[:], 0.0), writes=SKPb)
        S.dve(lambda: nc.vector.memset(SKT[:], 0.0), writes=[SKTb])
        S.dve(lambda: nc.vector.memset(MQT[:], 0.0), writes=[MQTb])

    def prologue(ti):
        ns = None
        S.dma("sp", lambda: nc.sync.dma_start(out=XS, in_=x_d[ti * T:(ti + 1) * T, :].rearrange("(b p) d -> p b d", p=128)),
              XSown, writes=XSl)
        for kc in range(KC):
            if kc == 0:
                ns = norm_begin((0, 0))
            bk, bb = nb()
            def tr(kc=kc, bk=bk):
                for b in range(NBLK):
                    ins = nc.tensor.transpose(bk[:, b * 128:(b + 1) * 128], XS[:, b, kc * 128:(kc + 1) * 128], identf[:])
                return ins
            S.pe(tr, reads=XSl + [CONb], writes=[bb])
            if kc % 2 == 0:
                S.act(lambda kc=kc, bk=bk: nc.scalar.copy(out=XT[:, kc, :], in_=bk[:]), reads=[bb], writes=[XTb[kc]])
            else:
                S.dve(lambda kc=kc, bk=bk: nc.vector.tensor_copy(out=XT[:, kc, :], in_=bk[:]), reads=[bb], writes=[XTb[kc]])
            fb(bb)
            norm_feed(ns, kc)
        return ns

    def epilogue(ti):
        for b in range(NBLK):
            for half in range(2):
                bk, bb = nb()
                def tr(b=b, half=half, bk=bk):
                    for q in range(4):
                        ins = nc.tensor.transpose(bk[:, q * 128:(q + 1) * 128], XT[:, half * 4 + q, b * 128:(b + 1) * 128], identf[:])
                    return ins
                S.pe(tr, reads=XTb[half * 4:half * 4 + 4] + [CONb], writes=[bb])
                if half == 0:
                    S.act(lambda b=b, half=half, bk=bk: nc.scalar.copy(out=XS[:, b, half * 512:(half + 1) * 512], in_=bk[:]), reads=[bb], writes=XSl)
                else:
                    S.dve(lambda b=b, half=half, bk=bk: nc.vector.tensor_copy(out=XS[:, b, half * 512:(half + 1) * 512], in_=bk[:]), reads=[bb], writes=XSl)
                fb(bb)
        S.dma("sp", lambda: nc.sync.dma_start(out=out_d[ti * T:(ti + 1) * T, :].rearrange("(b p) d -> p b d", p=128), in_=XS),
              XSown, reads=XSl)

    def rstd_from(bk, bb, scale):
        t1, t1b = tmpA.get()
        S.act(lambda: nc.scalar.activation(out=t1[:], in_=bk[:], func=AF.Ln, scale=scale, bias=EPS_AP[:]), reads=[bb, CONb], writes=[t1b])
        t2, t2b = tmpA.get()
        S.act(lambda: nc.scalar.activation(out=t2[:], in_=t1[:], func=AF.Exp, scale=-0.5), reads=[t1b], writes=[t2b])
        return t2, t2b

    RS = sb("RS", [128, T]); RSb = Buf("RS")

    def norm_begin(defer=None):
        bk, bb = nb()
        return dict(bk=bk, bb=bb, pend=[], n=0, defer=defer)

    def norm_flush(ns, keep=0):
        while len(ns["pend"]) > keep:
            sq, sqb = ns["pend"].pop(0)
            k = ns["n"]
            bk, bb = ns["bk"], ns["bb"]
            S.pe(lambda sq=sq, k=k, bk=bk: nc.tensor.matmul(bk[:], lhsT=onesb[:], rhs=sq[:], start=(k == 0), stop=(k == KC - 1)), reads=[sqb, CONb], writes=[bb])
            ns["n"] += 1

    def norm_feed(ns, c):
        if ns is None:
            return
        sq, sqb = tmpH.get()
        S.act(lambda: nc.scalar.activation(out=sq[:], in_=XT[:, c, :], func=AF.Square), reads=[XTb[c]], writes=[sqb])
        if ns["defer"] is not None:
            l_, w_ = ns["defer"]
            g_ = off["ng"] + (l_ * 4 + w_) * 8 + c
            S.dve(lambda: nc.vector.tensor_scalar(out=XN[:, c, :], in0=XT[:, c, :], scalar1=PAR[:, g_:g_ + 1], scalar2=None, op0=ALU.mult),
                  reads=[XTb[c], PARb], writes=[XNb])
        ns["pend"].append((sq, sqb))
        norm_flush(ns, keep=2)

    def norm_end(ns, l, which):
        norm_flush(ns)
        assert ns["n"] == KC
        bk, bb = ns["bk"], ns["bb"]
        if ns["defer"] is not None:
            assert ns["defer"] == (l, which)
            t1, t1b = tmpA.get()
            S.act(lambda: nc.scalar.activation(out=t1[:], in_=bk[:], func=AF.Ln, scale=1.0 / D, bias=EPS_AP[:]), reads=[bb, CONb], writes=[t1b])
            S.act(lambda: nc.scalar.activation(out=RS[:], in_=t1[:], func=AF.Exp, scale=-0.5), reads=[t1b], writes=[RSb])
            fb(bb)
            return True
        rs, rsb = rstd_from(bk, bb, 1.0 / D)
        fb(bb)
        g0 = off["ng"] + (l * 4 + which) * 8
        def sc_():
            for kc in range(KC):
                ins = nc.vector.scalar_tensor_tensor(out=XN[:, kc, :], in0=XT[:, kc, :], scalar=PAR[:, g0 + kc:g0 + kc + 1], in1=rs[:],
                                                     op0=ALU.mult, op1=ALU.mult)
            return ins
        S.dve(sc_, reads=XTb + [rsb, PARb], writes=[XNb])
        return False

    def ffn(l, f, ns_in, ti=None):
        if f == 1:
            S.dma("sp", lambda: nc.sync.dma_start(out=PS_[:], in_=p_d[l, ti * T:(ti + 1) * T, :].rearrange("(b p) d -> p b d", p=128)), PSb_, writes=[PSb_])
        dfr = norm_end(ns_in, l, 0 if f == 0 else 2)
        ns = None
        for j in range(NJ):
            rt, rb, n = w_next(f"wi{f}_{j}")
            bg, bgb = nb()
            bu, bub = nb()
            def mmg(rt=rt, bg=bg):
                for kc in range(KC):
                    ins = nc.tensor.matmul(bg[:], lhsT=rt[:, kc * 256:kc * 256 + 128], rhs=XN[:, kc, :], start=(kc == 0), stop=(kc == KC - 1))
                return ins
            def mmu(rt=rt, bu=bu):
                for kc in range(KC):
                    ins = nc.tensor.matmul(bu[:], lhsT=rt[:, kc * 256 + 128:kc * 256 + 256], rhs=XN[:, kc, :], start=(kc == 0), stop=(kc == KC - 1))
                return ins
            S.pe(mmg, reads=[rb, XNb], writes=[bgb])
            S.pe(mmu, reads=[rb, XNb], writes=[bub])
            w_done(n)
            sg, sgb = tmpA.get()
            if dfr:
                gs_, gsb_ = tmpB.get()
                S.dve(lambda gs_=gs_, bg=bg: nc.vector.tensor_tensor(out=gs_[:], in0=bg[:], in1=RS[:], op=ALU.mult), reads=[bgb, RSb], writes=[gsb_])
                S.act(lambda sg=sg, gs_=gs_: nc.scalar.activation(out=sg[:], in_=gs_[:], func=AF.Silu), reads=[gsb_], writes=[sgb])
                us_, usb_ = tmpB.get()
                S.dve(lambda us_=us_, bu=bu: nc.vector.tensor_tensor(out=us_[:], in0=bu[:], in1=RS[:], op=ALU.mult), reads=[bub, RSb], writes=[usb_])
                S.dve(lambda j=j, us_=us_, sg=sg: nc.vector.tensor_tensor(out=H[:, j, :], in0=us_[:], in1=sg[:], op=ALU.mult),
                      reads=[usb_, sgb], writes=[Hb[j]])
            else:
                S.act(lambda sg=sg, bg=bg: nc.scalar.activation(out=sg[:], in_=bg[:], func=AF.Silu), reads=[bgb], writes=[sgb])
                S.dve(lambda j=j, bu=bu, sg=sg: nc.vector.tensor_tensor(out=H[:, j, :], in0=bu[:], in1=sg[:], op=ALU.mult),
                      reads=[bub, sgb], writes=[Hb[j]])
            fb(bgb, bub)
        for c in range(8):
            rt, rb, n = w_next(f"wo{f}_{c}")
            if c == 0:
                ns = norm_begin((l, 3) if f == 1 else None)
            bk, bb = nb()
            def mmo(rt=rt, bk=bk):
                for j in range(NJ):
                    ins = nc.tensor.matmul(bk[:], lhsT=rt[:, j * 128:(j + 1) * 128], rhs=H[:, j, :], start=(j == 0), stop=(j == NJ - 1))
                return ins
            S.pe(mmo, reads=[rb] + Hb, writes=[bb])
            w_done(n)
            S.dve(lambda c=c, bk=bk: nc.vector.scalar_tensor_tensor(out=XT[:, c, :], in0=bk[:], scalar=0.5, in1=XT[:, c, :], op0=ALU.mult, op1=ALU.add),
                  reads=[bb, XTb[c]], writes=[XTb[c]])
            fb(bb)
            norm_feed(ns, c)
        return ns

    def projA(rt, rb, i, bk, bb):
        def mm():
            for kc in range(KC):
                ins = nc.tensor.matmul(bk[:], lhsT=rt[:, (i * 8 + kc) * 128:(i * 8 + kc + 1) * 128], rhs=XN[:, kc, :], start=(kc == 0), stop=(kc == KC - 1))
            return ins
        S.pe(mm, reads=[rb, XNb], writes=[bb])

    def qk_wave_start(rt, rb):
        raws = []
        for i in range(2):
            bk, bb = nb()
            projA(rt, rb, i, bk, bb)
            sq, sqb = tmpH.get()
            S.act(lambda sq=sq, bk=bk: nc.scalar.activation(out=sq[:], in_=bk[:], func=AF.Square), reads=[bb], writes=[sqb])
            raws.append((bk, bb, sq, sqb))
        return raws

    def qk_wave_finish(raws, fin, dstb):
        for i in range(2):
            bk, bb, sq, sqb = raws[i]
            b2, b2b = nb()
            S.pe(lambda b2=b2, sq=sq: nc.tensor.matmul(b2[:], lhsT=blockones[:], rhs=sq[:], start=True, stop=True), reads=[sqb, CONb], writes=[b2b])
            rs, rsb = rstd_from(b2, b2b, 1.0 / 64)
            fb(b2b)
            S.dve(lambda i=i, bk=bk, rs=rs: fin(i, bk, rs), reads=[bb, rsb, CONb, PARb], writes=[dstb])
            fb(bb)

    def mixer(ti, l, ns_in):
        norm_end(ns_in, l, 1)
        ns = None
        S.dma("sp", lambda: nc.sync.dma_start(out=WB[:].rearrange("p k c -> p (k c)"), in_=sc[l]["winB"]), WBb,
              reads=[sc[l]["b"]["winB"]], writes=[WBb])
        rt, rb, n = w_next("winA0")
        raws = qk_wave_start(rt, rb)
        w_done(n)
        rt, rb, n = w_next("winA1")
        for i in range(2):
            bk, bb = nb()
            projA(rt, rb, i, bk, bb)
            def fmq(i=i, bk=bk):
                nc.scalar.mul(out=MQT[0:64, 2 * i, :], in_=bk[0:64, :], mul=0.125)
                return nc.scalar.mul(out=MQT[64:128, 2 * i + 1, :], in_=bk[64:128, :], mul=0.125)
            S.act(fmq, reads=[bb], writes=[MQTb])
            fb(bb)
        w_done(n)
        def fin_q(c0):
            return lambda i, bk, rs: nc.vector.scalar_tensor_tensor(out=SQT[:, c0 + i, :], in0=bk[:], scalar=GQ8[:, l:l + 1], in1=rs[:], op0=ALU.mult, op1=ALU.mult)
        qk_wave_finish(raws, fin_q(0), SQTb)
        rt, rb, n = w_next("winA2")
        raws = qk_wave_start(rt, rb)
        w_done(n)
        rt, rb, n = w_next("winA3")
        for i in range(2):
            bk, bb = nb()
            projA(rt, rb, i, bk, bb)
            S.dve(lambda i=i, bk=bk: nc.vector.tensor_copy(out=MKT[:, i, :], in_=bk[:]), reads=[bb], writes=[MKTb])
            fb(bb)
        w_done(n)
        qk_wave_finish(raws, fin_q(2), SQTb)
        rt, rb, n = w_next("winA4")
        raws = qk_wave_start(rt, rb)
        w_done(n)
        gk0 = off["gqk"] + 2 * l + 1
        def fin_k(g, bk, rs):
            nc.vector.scalar_tensor_tensor(out=SKT[0:64, g, 0, :], in0=bk[0:64, :], scalar=PAR[0:64, gk0:gk0 + 1], in1=rs[0:64, :], op0=ALU.mult, op1=ALU.mult)
            return nc.vector.scalar_tensor_tensor(out=SKT[64:128, g, 1, :], in0=bk[64:128, :], scalar=PAR[64:128, gk0:gk0 + 1], in1=rs[64:128, :], op0=ALU.mult, op1=ALU.mult)
        qk_wave_finish(raws, fin_k, SKTb)

        cxs = [mixer_stage1(ti, l, b) for b in range(NBLK)]
        for b in range(NBLK):
            mixer_stage2(ti, l, b, cxs[b])
        for b in range(NBLK):
            mixer_stage2b(ti, l, b, cxs[b])
            if b > 0:
                mixer_stage2d(ti, l, b - 1, cxs[b - 1])
        mixer_stage2d(ti, l, NBLK - 1, cxs[NBLK - 1])

        S.act(lambda: nc.scalar.copy(out=SKP[:, l], in_=SKT[:, :, :, (NBLK - 1) * 128:NBLK * 128]), reads=[SKTb], writes=[SKPb[l]])
        S.act(lambda: nc.scalar.copy(out=SVP[:, l], in_=SVA[:, NBLK - 1]), reads=[SVAb[NBLK - 1]], writes=[SVPb[l]])

        for c in range(8):
            rt, rb, n = w_next(f"mix{c}")
            bga, bgab = nb(); bya, byab = nb(); bgb_, bgbb = nb(); byb, bybb = nb()
            def mm8(bk, o, rt=rt):
                def f_():
                    for kc in range(KC):
                        ins = nc.tensor.matmul(bk[:], lhsT=rt[:, o + kc * 128:o + (kc + 1) * 128], rhs=XN[:, kc, :], start=(kc == 0), stop=(kc == KC - 1))
                    return ins
                return f_
            def mm4(bk, o, src, rt=rt):
                def f_():
                    for kc in range(4):
                        ins = nc.tensor.matmul(bk[:], lhsT=rt[:, o + kc * 128:o + (kc + 1) * 128], rhs=src(kc), start=(kc == 0), stop=(kc == 3))
                    return ins
                return f_
            S.pe(mm8(bga, 0), reads=[rb, XNb], writes=[bgab])
            S.pe(mm4(bya, 2048, lambda k: HAGT(k)), reads=[rb] + Hb[16:20], writes=[byab])
            S.pe(mm8(bgb_, 1024), reads=[rb, XNb], writes=[bgbb])
            S.pe(mm4(byb, 2560, lambda k: HBT[:, k, :]), reads=[rb, HBTb], writes=[bybb])
            w_done(n)
            sa, sab = tmpA.get()
            S.act(lambda sa=sa, bga=bga: nc.scalar.activation(out=sa[:], in_=bga[:], func=AF.Sigmoid), reads=[bgab], writes=[sab])
            sb_, sbb = tmpA.get()
            S.act(lambda sb_=sb_, bgb_=bgb_: nc.scalar.activation(out=sb_[:], in_=bgb_[:], func=AF.Sigmoid), reads=[bgbb], writes=[sbb])
            m1, m1b = tmpB.get()
            S.dve(lambda m1=m1, bya=bya, sa=sa: nc.vector.tensor_tensor(out=m1[:], in0=bya[:], in1=sa[:], op=ALU.mult), reads=[byab, sab], writes=[m1b])
            m2, m2b = tmpB.get()
            S.dve(lambda m2=m2, byb=byb, sb_=sb_: nc.vector.tensor_tensor(out=m2[:], in0=byb[:], in1=sb_[:], op=ALU.mult), reads=[bybb, sbb], writes=[m2b])
            S.dve(lambda c=c, m1=m1, m2=m2: nc.vector.tensor_tensor(out=MIXT(c), in0=m1[:], in1=m2[:], op=ALU.add), reads=[m1b, m2b], writes=[Hb[8 + c]])
            fb(bgab, byab, bgbb, bybb)
        for c in range(8):
            rt, rb, n = w_next(f"wout{c}")
            if c == 0:
                ns = norm_begin((l, 2))
            bk, bb = nb()
            def mmw(rt=rt, bk=bk):
                for kc in range(KC):
                    ins = nc.tensor.matmul(bk[:], lhsT=rt[:, kc * 128:(kc + 1) * 128], rhs=MIXT(kc), start=(kc == 0), stop=(kc == KC - 1))
                return ins
            S.pe(mmw, reads=[rb] + Hb[8:16], writes=[bb])
            w_done(n)
            S.dve(lambda c=c, bk=bk: nc.vector.tensor_tensor(out=XT[:, c, :], in0=bk[:], in1=XT[:, c, :], op=ALU.add), reads=[bb, XTb[c]], writes=[XTb[c]])
            fb(bb)
            norm_feed(ns, c)
        return ns

    def mixer_stage1(ti, l, b):
        gb = ti * NBLK + b
        bc = slice(b * 128, (b + 1) * 128)
        bmisc, bmiscb = nb(); bmv, bmvb = nb(); bmo, bmob = nb()
        def mmB(bk, c0, n_):
            def f_():
                for kc in range(KC):
                    ins = nc.tensor.matmul(bk[:, 0:n_], lhsT=XN[:, kc, bc], rhs=WB[:, kc, c0:c0 + n_], start=(kc == 0), stop=(kc == KC - 1))
                return ins
            return f_
        S.pe(mmB(bmisc, 1024, 392), reads=[XNb, WBb], writes=[bmiscb])
        S.pe(mmB(bmv, 0, 512), reads=[XNb, WBb], writes=[bmvb])
        S.pe(mmB(bmo, 512, 512), reads=[XNb, WBb], writes=[bmob])
        sm = lambda: SM.get()
        gt, gtb = sm()
        g0 = off["bg"] + l * 8
        S.dve(lambda: nc.vector.tensor_tensor(out=gt[:, 0:8], in0=bmisc[:, 256:264], in1=PAR[:, g0:g0 + 8], op=ALU.add), reads=[bmiscb, PARb], writes=[gtb])
        e1, e1b = sm()
        S.act(lambda: nc.scalar.activation(out=e1[:, 0:4], in_=gt[:, 4:8], func=AF.Exp, scale=-1.0), reads=[gtb], writes=[e1b])
        l1, l1b = sm()
        S.act(lambda: nc.scalar.activation(out=l1[:, 0:4], in_=e1[:, 0:4], func=AF.Ln, bias=ONE_AP[:]), reads=[e1b, CONb], writes=[l1b])
        mk, mkb = MKK.get()
        S.dve(lambda: nc.vector.tensor_copy(out=mk[:], in_=bmisc[:, 0:256]), reads=[bmiscb], writes=[mkb])
        S.act(lambda: nc.scalar.copy(out=SVA[:, b, :, 0:64], in_=bmisc[:, 264:392].rearrange("p (g d) -> p g d", g=2)), reads=[bmiscb], writes=[SVAb[b]])
        mvs, mvsb = MVs.get()
        S.act(lambda: nc.scalar.copy(out=mvs[:], in_=bmv[:]), reads=[bmvb], writes=[mvsb])
        smo, smob = SMO.get()
        S.act(lambda: nc.scalar.activation(out=smo[:], in_=bmo[:], func=AF.Sigmoid), reads=[bmob], writes=[smob])
        fb(bmiscb, bmvb, bmob)
        bgt, bgtb = nb()
        def mmg():
            nc.tensor.matmul(bgt[:, 0:4], lhsT=tri4[:, 0:128], rhs=l1[:, 0:4], start=True, stop=True)
            return nc.tensor.matmul(bgt[:, 8:12], lhsT=onesf[:], rhs=l1[:, 0:4], start=True, stop=True)
        S.pe(mmg, reads=[l1b, CONb], writes=[bgtb])
        a1, a1b = sm()
        S.dve(lambda: nc.vector.tensor_tensor(out=a1[:, 0:4], in0=bgt[:, 0:4], in1=gt[:, 0:4], op=ALU.add), reads=[bgtb, gtb], writes=[a1b])
        cc, ccb = sm()
        S.act(lambda: nc.scalar.activation(out=cc[:, 0:4], in_=a1[:, 0:4], func=AF.Exp), reads=[a1b], writes=[ccb])
        fl, flb = sm()
        S.act(lambda: nc.scalar.activation(out=fl[:, 0:4], in_=bgt[:, 0:4], func=AF.Exp), reads=[bgtb], writes=[flb])
        eg, egb = sm()
        def feg():
            nc.scalar.activation(out=eg[0:64, 0:2], in_=bgt[0:64, 8:12:2], func=AF.Exp, scale=-1.0)
            return nc.scalar.activation(out=eg[64:128, 0:2], in_=bgt[64:128, 9:13:2], func=AF.Exp, scale=-1.0)
        S.act(feg, reads=[bgtb], writes=[egb])
        fb(bgtb)
        vpp, vppb = VPP.get()
        def fv():
            for h in range(4):
                nc.vector.tensor_scalar(out=vpp[:, h, 0:128], in0=mvs[:, h * 128:(h + 1) * 128], scalar1=cc[:, h:h + 1], scalar2=None, op0=ALU.mult)
            return nc.vector.tensor_copy(out=vpp[:, :, 128], in_=cc[:, 0:4])
        S.dve(fv, reads=[mvsb, ccb], writes=[vppb])
        gs, gsb = GSP.get()
        m0 = off["mlg"] + l * 512
        S.dve(lambda: nc.vector.tensor_tensor(out=gs[:], in0=smo[:], in1=PAR[:, m0:m0 + 512], op=ALU.mult), reads=[smob, PARb], writes=[gsb])

        return dict(cc=cc, ccb=ccb, fl=fl, flb=flb, eg=eg, egb=egb, vpp=vpp, vppb=vppb, gs=gs, gsb=gsb, mk=mk, mkb=mkb)

    def mixer_stage2(ti, l, b, cx):
        gb = ti * NBLK + b
        bc = slice(b * 128, (b + 1) * 128)
        sm = lambda: SM.get()
        bst, bstb = nb()
        def mms():
            for h in range(4):
                hp = (h % 2) * 64
                ins = nc.tensor.matmul(bst[:, h * 128:(h + 1) * 128], lhsT=MKT[:, h // 2, bc], rhs=MQT[:, h, bc], start=True, stop=True)
            return ins
        S.pe(mms, reads=[MKTb, MQTb], writes=[bstb])
        ptm, ptmb = PTM.get()
        S.dve(lambda: nc.vector.tensor_tensor(out=ptm[:], in0=bst[:], in1=tri4[:], op=ALU.mult), reads=[bstb, CONb], writes=[ptmb])
        fb(bstb)
        cx["ptm"] = ptm
        cx["ptmb"] = ptmb

    def mixer_stage2b(ti, l, b, cx):
        gb = ti * NBLK + b
        bc = slice(b * 128, (b + 1) * 128)
        sm = lambda: SM.get()
        fl, flb, eg, egb, vpp, vppb, gs, gsb, mk, mkb, ptm, ptmb = (cx[k] for k in ("fl", "flb", "eg", "egb", "vpp", "vppb", "gs", "gsb", "mk", "mkb", "ptm", "ptmb"))
        kbs = [1] if gb == 0 else [0, 1]
        pts = {}
        for g in range(2):
            for kb in kbs:
                bss, bssb = nb()
                def mmss(g=g, kb=kb, bss=bss):
                    for i in range(4):
                        hq = 4 * g + i
                        hf = hq % 2
                        if kb == 1:
                            kt = SKT[:, g, hf, bc]
                        elif b > 0:
                            kt = SKT[:, g, hf, (b - 1) * 128:b * 128]
                        else:
                            kt = SKP[:, l, g, hf, :]
                        ins = nc.tensor.matmul(bss[:, i * 128:(i + 1) * 128], lhsT=kt, rhs=SQT[:, hq // 2, bc], start=True, stop=True)
                    return ins
                S.pe(mmss, reads=[SKTb, SKPb[l], SQTb], writes=[bssb])
                ee, eeb = tmpA.get()
                S.act(lambda ee=ee, bss=bss: nc.scalar.activation(out=ee[:], in_=bss[:], func=AF.Exp), reads=[bssb], writes=[eeb])
                fb(bssb)
                pt, ptb = PTS.get()
                S.dve(lambda pt=pt, ee=ee, g=g, kb=kb: nc.vector.tensor_tensor(out=pt[:], in0=ee[:], in1=EB[:, kb, 4 * g:4 * g + 4, :].rearrange("p h t -> p (h t)"), op=ALU.mult),
                      reads=[eeb, CONb], writes=[ptb])
                pts[(g, kb)] = (pt, ptb)
        bn = [nb(), nb()]
        def mmn():
            for h in range(4):
                hp = (h % 2) * 64
                o = bn[h // 2][0][:, (h % 2) * 129:(h % 2) * 129 + 129]
                nc.tensor.matmul(o, lhsT=ptm[:, h * 128:(h + 1) * 128], rhs=vpp[:, h, :], start=True, stop=False)
                ins = nc.tensor.matmul(o, lhsT=MQT[:, h, bc], rhs=CSB[:, l, h // 2, :], start=False, stop=True)
            return ins
        S.pe(mmn, reads=[ptmb, vppb, MQTb, CSBb[l]], writes=[bn[0][1], bn[1][1]])
        bu = [nb(), nb()]
        def mmu():
            for i in range(2):
                ins = nc.tensor.matmul(bu[i][0][:, 0:258], lhsT=mk[:, i * 128:(i + 1) * 128], rhs=vpp[:, 2 * i:2 * i + 2, :].rearrange("p a b -> p (a b)"), start=True, stop=True)
            return ins
        S.pe(mmu, reads=[mkb, vppb], writes=[bu[0][1], bu[1][1]])
        for i in range(2):
            def fadd(i=i):
                nc.vector.tensor_tensor(out=CS[0:64, l, i, :], in0=bu[i][0][0:64, 0:129], in1=CS[0:64, l, i, :], op=ALU.add)
                return nc.vector.tensor_tensor(out=CS[64:128, l, i, :], in0=bu[i][0][64:128, 129:258], in1=CS[64:128, l, i, :], op=ALU.add)
            S.dve(fadd, reads=[bu[i][1], CSb[l]], writes=[CSb[l]])
            S.dve(lambda i=i: nc.vector.tensor_scalar(out=CS[:, l, i, :], in0=CS[:, l, i, :], scalar1=eg[:, i:i + 1], scalar2=None, op0=ALU.mult),
                  reads=[egb, CSb[l]], writes=[CSb[l]])
        fb(bu[0][1], bu[1][1])
        S.act(lambda: nc.scalar.copy(out=CSB[:, l], in_=CS[:, l]), reads=[CSb[l]], writes=[CSBb[l]])
        dn, dnb = sm()
        ad, adb = sm()
        def fad():
            for i in range(2):
                ins = nc.scalar.activation(out=ad[:, 2 * i:2 * i + 2], in_=bn[i][0][:, 128:258:129], func=AF.Abs)
            return ins
        S.act(fad, reads=[bn[0][1], bn[1][1]], writes=[adb])
        S.dve(lambda: nc.vector.tensor_tensor(out=dn[:, 0:4], in0=ad[:, 0:4], in1=fl[:, 0:4], op=ALU.max), reads=[adb, flb], writes=[dnb])
        nss, nssb = sm()
        junk, junkb = tmpH.get()
        def fss():
            for h in range(4):
                o = (h % 2) * 129
                ins = nc.scalar.activation(out=junk[:, h * 128:(h + 1) * 128], in_=bn[h // 2][0][:, o:o + 128], func=AF.Square, accum_out=nss[:, h:h + 1])
            return ins
        S.act(fss, reads=[bn[0][1], bn[1][1]], writes=[nssb, junkb])
        t2, t2b = sm()
        S.dve(lambda: nc.vector.scalar_tensor_tensor(out=t2[:, 0:4], in0=dn[:, 0:4], scalar=EPS, in1=dn[:, 0:4], op0=ALU.mult, op1=ALU.mult), reads=[dnb], writes=[t2b])
        t3, t3b = sm()
        S.dve(lambda: nc.vector.scalar_tensor_tensor(out=t3[:, 0:4], in0=nss[:, 0:4], scalar=1.0 / 128, in1=t2[:, 0:4], op0=ALU.mult, op1=ALU.add), reads=[t2b, nssb], writes=[t3b])
        ln2, ln2b = sm()
        S.act(lambda: nc.scalar.activation(out=ln2[:, 0:4], in_=t3[:, 0:4], func=AF.Ln), reads=[t3b], writes=[ln2b])
        fac, facb = sm()
        S.act(lambda: nc.scalar.activation(out=fac[:, 0:4], in_=ln2[:, 0:4], func=AF.Exp, scale=-0.5), reads=[ln2b], writes=[facb])
        htk, htkb = HTK.get()
        def fh():
            for h in range(4):
                o = (h % 2) * 129
                ins = nc.vector.scalar_tensor_tensor(out=htk[:, h * 128:(h + 1) * 128], in0=bn[h // 2][0][:, o:o + 128], scalar=fac[:, h:h + 1],
                                                     in1=gs[:, h * 128:(h + 1) * 128], op0=ALU.mult, op1=ALU.mult)
            return ins
        S.dve(fh, reads=[bn[0][1], bn[1][1], facb, gsb], writes=[htkb])
        fb(bn[0][1], bn[1][1])
        bo = [nb(), nb()]
        def mmo():
            for hq in range(8):
                g, i = hq // 4, hq % 4
                o = bo[g][0][:, i * 65:(i + 1) * 65]
                if 0 in kbs:
                    vprev = SVA[:, b - 1, g, :] if b > 0 else SVP[:, l, g, :]
                    nc.tensor.matmul(o, lhsT=pts[(g, 0)][0][:, i * 128:(i + 1) * 128], rhs=vprev, start=True, stop=False)
                ins = nc.tensor.matmul(o, lhsT=pts[(g, 1)][0][:, i * 128:(i + 1) * 128], rhs=SVA[:, b, g, :], start=(0 not in kbs), stop=True)
            return ins
        S.pe(mmo, reads=[v[1] for v in pts.values()] + [SVAb[b], SVAb[(b - 1) % NBLK], SVPb[l]], writes=[bo[0][1], bo[1][1]])
        d8, d8b = sm()
        def fd8():
            for g in range(2):
                ins = nc.vector.tensor_tensor(out=d8[:, 4 * g:4 * g + 4], in0=bo[g][0][:, 64:260:65], in1=ES[:, l * 8 + 4 * g:l * 8 + 4 * g + 4], op=ALU.add)
            return ins
        S.dve(fd8, reads=[bo[0][1], bo[1][1], CONb], writes=[d8b])
        r8, r8b = sm()
        S.dve(lambda: nc.vector.reciprocal(out=r8[:, 0:8], in_=d8[:, 0:8]), reads=[d8b], writes=[r8b])
        hbk, hbkb = HTK.get()
        def fhb():
            for hq in range(8):
                g, i = hq // 4, hq % 4
                ins = nc.vector.tensor_scalar(out=hbk[:, hq * 64:(hq + 1) * 64], in0=bo[g][0][:, i * 65:i * 65 + 64], scalar1=r8[:, hq:hq + 1], scalar2=None, op0=ALU.mult)
            return ins
        S.dve(fhb, reads=[bo[0][1], bo[1][1], r8b], writes=[hbkb])
        fb(bo[0][1], bo[1][1])
        cx['htk'] = htk; cx['htkb'] = htkb; cx['hbk'] = hbk; cx['hbkb'] = hbkb

    def mixer_stage2d(ti, l, b, cx):
        bc = slice(b * 128, (b + 1) * 128)
        htk, htkb, hbk, hbkb = cx['htk'], cx['htkb'], cx['hbk'], cx['hbkb']
        def ftr2():
            for k in range(4):
                ins = nc.tensor.transpose(bankT[:, 512 + k * 128:512 + (k + 1) * 128], hbk[:, k * 128:(k + 1) * 128], identb[:])
            return ins
        def ftr():
            for h in range(4):
                ins = nc.tensor.transpose(bankT[:, h * 128:(h + 1) * 128], htk[:, h * 128:(h + 1) * 128], identb[:])
            return ins
        S.pe(ftr, reads=[htkb, CONb], writes=[bankTb[0]])
        S.act(lambda: nc.scalar.copy(out=H[:, 16:20, bc], in_=bankT[:, 0:512].rearrange("p (h t) -> p h t", h=4)), reads=[bankTb[0]], writes=Hb[16:20])

        S.pe(ftr2, reads=[hbkb, CONb], writes=[bankTb[1]])
        S.dve(lambda: nc.vector.tensor_copy(out=HBT[:, :, bc], in_=bankT[:, 512:1024].rearrange("p (h t) -> p h t", h=4)), reads=[bankTb[1]], writes=[HBTb])

    def ple(ti, l, ns_in, want_next):
        for k in range(2):
            bk, bb = nb()
            def tr(k=k, bk=bk):
                for b in range(NBLK):
                    ins = nc.tensor.transpose(bk[:, b * 128:(b + 1) * 128], PS_[:, b, k * 128:(k + 1) * 128], identf[:])
                return ins
            S.pe(tr, reads=[PSb_, CONb], writes=[bb])
            S.act(lambda k=k, bk=bk: nc.scalar.copy(out=PT[:, k, :], in_=bk[:]), reads=[bb], writes=[PTb])
            fb(bb)
        dfr = norm_end(ns_in, l, 3)
        ns = norm_begin() if want_next else None
        for c in range(8):
            rt, rb, n = w_next(f"ple{c}")
            bg, bgb = nb(); bp, bpb = nb()
            def mmg(rt=rt, bg=bg):
                for kc in range(KC):
                    ins = nc.tensor.matmul(bg[:], lhsT=rt[:, kc * 128:(kc + 1) * 128], rhs=XN[:, kc, :], start=(kc == 0), stop=(kc == KC - 1))
                return ins
            def mmp(rt=rt, bp=bp):
                for k in range(2):
                    ins = nc.tensor.matmul(bp[:], lhsT=rt[:, 1024 + k * 128:1024 + (k + 1) * 128], rhs=PT[:, k, :], start=(k == 0), stop=(k == 1))
                return ins
            S.pe(mmg, reads=[rb, XNb], writes=[bgb])
            S.pe(mmp, reads=[rb, PTb], writes=[bpb])
            w_done(n)
            sg, sgb = tmpA.get()
            if dfr:
                gs_, gsb_ = tmpB.get()
                S.dve(lambda gs_=gs_, bg=bg: nc.vector.tensor_tensor(out=gs_[:], in0=bg[:], in1=RS[:], op=ALU.mult), reads=[bgb, RSb], writes=[gsb_])
                S.act(lambda sg=sg, gs_=gs_: nc.scalar.activation(out=sg[:], in_=gs_[:], func=AF.Sigmoid), reads=[gsb_], writes=[sgb])
            else:
                S.act(lambda sg=sg, bg=bg: nc.scalar.activation(out=sg[:], in_=bg[:], func=AF.Sigmoid), reads=[bgb], writes=[sgb])
            m, mb = tmpB.get()
            S.dve(lambda m=m, bp=bp, sg=sg: nc.vector.tensor_tensor(out=m[:], in0=bp[:], in1=sg[:], op=ALU.mult), reads=[bpb, sgb], writes=[mb])
            S.dve(lambda c=c, m=m: nc.vector.tensor_tensor(out=XT[:, c, :], in0=m[:], in1=XT[:, c, :], op=ALU.add), reads=[mb, XTb[c]], writes=[XTb[c]])
            fb(bgb, bpb)
            norm_feed(ns, c)
        return ns

    EPS_AP = sb("EPS_AP", [128, 1]); ONE_AP = sb("ONE_AP", [128, 1])
    S.dve(lambda: nc.vector.memset(EPS_AP[:], EPS), writes=[CONb])
    S.dve(lambda: nc.vector.memset(ONE_AP[:], 1.0), writes=[CONb])
    import os as _os
    KPH = _os.environ.get("KPH", "cfmp")
    for l in range(L):
        if "c" in KPH:
            emit_casts(l)
    setup()
    for ti in range(NT):
        ns = prologue(ti)
        for l in range(L):
            ns = ffn(l, 0, ns)
            ns = mixer(ti, l, ns)
            ns = ffn(l, 1, ns, ti)
            ns = ple(ti, l, ns, l < L - 1)
        epilogue(ti)
    S.final_wait("sp", XSl)
    print("sbuf bytes remaining", nc.sbuf_bytes_remaining)
    S.emit()
    return nc


_CACHE = {}


def kernel(x, p, ffn1_norm, ffn1_wi, ffn1_wo, mix_norm, w_in, b_igate, b_fgate, ml_out_norm,
           q_norm, k_norm, sinks, rel_bias, w_a, w_b, w_out, ffn2_norm, ffn2_wi, ffn2_wo,
           ple_norm, w_ple_gate, w_ple):
    f = lambda a: np.ascontiguousarray(np.asarray(a, dtype=np.float32))
    x = f(x); p = f(p)
    B, S_LEN, _ = x.shape
    L = p.shape[0]
    key = (S_LEN, L)
    if key not in _CACHE:
        _CACHE[key] = build_program(S_LEN, L)
    nc = _CACHE[key]
    params = pack_params(L, f(ffn1_norm), f(mix_norm), f(ffn2_norm), f(ple_norm), f(b_igate), f(b_fgate),
                         f(ml_out_norm), f(q_norm), f(k_norm), f(sinks))
    consts = make_consts(f(rel_bias))
    shared = {"ffn1_wi": f(ffn1_wi), "ffn2_wi": f(ffn2_wi), "ffn1_wo": f(ffn1_wo), "ffn2_wo": f(ffn2_wo),
              "w_in": f(w_in), "w_a": f(w_a), "w_b": f(w_b), "w_out": f(w_out), "w_ple_gate": f(w_ple_gate),
              "w_ple": f(w_ple), "params": params}
    shared.update(consts)
    in_maps = []
    for i in range(B):
        m = dict(shared)
        m["x"] = np.ascontiguousarray(x[i])
        m["p"] = np.ascontiguousarray(p[:, i])
        in_maps.append(m)
    res = run_bass_kernel_spmd(nc, in_maps, core_ids=list(range(B)))
    return np.stack([np.asarray(res.results[i]["out"], dtype=np.float32) for i in range(B)], axis=0)
```
